# Optimizing a Trainium2 kernel written in Bass

```python
import math
import jax
import jax.numpy as jnp
from jax import lax
import numpy as np

D_MODEL = 2048
BATCH = 4
SEQ = 2048
DEPTH = 1

N_META = 16
EPS = 1e-6

GLA_HEADS = 4
GLA_DK = D_MODEL // (2 * GLA_HEADS)
GLA_DV = D_MODEL // GLA_HEADS
GLA_QK = GLA_HEADS * GLA_DK
GLA_V = GLA_HEADS * GLA_DV
GLA_LOWRANK = 16
GLA_TAU = 16.0
GLA_CHUNK = 64
GLA_FRONT_PAD = GLA_CHUNK - N_META

DIFF_HEADS = 8
DIFF_DH = D_MODEL // (2 * DIFF_HEADS)
DIFF_DV = 2 * DIFF_DH
DIFF_QK = DIFF_HEADS * 2 * DIFF_DH
DIFF_V = DIFF_HEADS * DIFF_DV
Q_BLOCK = 128

ROPE_THETA = 500000.0
ROPE_DIMS = DIFF_DH // 4

PEER_HEADS = 8
PEER_NKEYS = 128
PEER_N = PEER_NKEYS * PEER_NKEYS
PEER_DKEY = 128
PEER_TOPK = 16
PEER_TOKEN_BLOCK = 64

IN_SIZES = (GLA_QK, GLA_QK, GLA_V, GLA_V, 2 * GLA_LOWRANK, DIFF_QK, DIFF_QK, DIFF_V, D_MODEL, D_MODEL)
IN_WIDTH = sum(IN_SIZES)

kernel_name = 'hybrid_gla_diffattn_peer_encoder'


def rmsnorm(x, g):
    xf = x.astype(jnp.float32)
    y = xf * lax.rsqrt(jnp.mean(xf * xf, axis=-1, keepdims=True) + EPS)
    return (y * g.astype(jnp.float32)).astype(x.dtype)


def split_points(sizes):
    pts, acc = [], 0
    for s in sizes[:-1]:
        acc += s
        pts.append(acc)
    return pts


def partial_rope(x, pos):
    half = ROPE_DIMS // 2
    inv = 1.0 / (ROPE_THETA ** (jnp.arange(half, dtype=jnp.float32) / half))
    ang = pos.astype(jnp.float32)[:, None] * inv[None, :]
    cos = jnp.cos(ang).astype(x.dtype)
    sin = jnp.sin(ang).astype(x.dtype)
    x1 = x[..., :half]
    x2 = x[..., half:ROPE_DIMS]
    return jnp.concatenate([x1 * cos - x2 * sin, x1 * sin + x2 * cos, x[..., ROPE_DIMS:]], axis=-1)


def gla_direction(q, k, v, logf, inclusive):
    C = q.shape[-2]
    b = jnp.cumsum(logf, axis=-2)
    b_last = b[..., -1:, :]
    q_in = q * jnp.exp(b)
    k_in = k * jnp.exp(-b)
    k_st = k * jnp.exp(b_last - b)
    mask = jnp.tril(jnp.ones((C, C), dtype=bool), 0 if inclusive else -1)
    a = jnp.where(mask, jnp.einsum('bhntd,bhnsd->bhnts', q_in, k_in), 0.0)
    o_intra = jnp.einsum('bhnts,bhnse->bhnte', a, v)
    d_state = jnp.einsum('bhnsd,bhnse->nbhde', k_st, v)
    decay = jnp.moveaxis(jnp.exp(b_last[..., 0, :]), 2, 0)

    def step(s, inp):
        dec, ds = inp
        return s * dec[..., None] + ds, s

    s0 = jnp.zeros(d_state.shape[1:], d_state.dtype)
    _, s_in = lax.scan(step, s0, (decay, d_state))
    o_inter = jnp.einsum('bhntd,nbhde->bhnte', q_in, s_in)
    return o_intra + o_inter


def gla_mixer(q, k, v, r, lr, w2_f, b_f, w2_b, b_b, g_norm):
    B, L, _ = q.shape
    C = GLA_CHUNK
    lr_f, lr_b = jnp.split(lr, 2, axis=-1)
    logf_f = jax.nn.log_sigmoid((lr_f @ w2_f + b_f).astype(jnp.float32)) / GLA_TAU
    logf_b = jax.nn.log_sigmoid((lr_b @ w2_b + b_b).astype(jnp.float32)) / GLA_TAU
    Lp = L + GLA_FRONT_PAD
    N = Lp // C

    def heads(t, dh):
        t = jnp.pad(t, ((0, 0), (GLA_FRONT_PAD, 0), (0, 0)))
        return t.reshape(B, N, C, GLA_HEADS, dh).transpose(0, 3, 1, 2, 4)

    qh = heads(q * (GLA_DK ** -0.5), GLA_DK)
    kh = heads(k, GLA_DK)
    vh = heads(v, GLA_DV)
    ff = heads(logf_f, GLA_DK)
    fb = heads(logf_b, GLA_DK)
    flip = lambda t: jnp.flip(t, axis=(2, 3))
    o = gla_direction(qh, kh, vh, ff, True) + flip(gla_direction(flip(qh), flip(kh), flip(vh), flip(fb), False))
    o = o.transpose(0, 2, 3, 1, 4).reshape(B, Lp, GLA_HEADS, GLA_DV)[:, GLA_FRONT_PAD:]
    o = rmsnorm(o, g_norm).reshape(B, L, GLA_V)
    return (o * jax.nn.silu(r.astype(jnp.float32))).astype(v.dtype)


def diff_attention(q, k, v, pos, lq1, lk1, lq2, lk2, g_norm, lam_init):
    B, L, _ = q.shape
    qh = partial_rope(q.reshape(B, L, DIFF_HEADS, 2, DIFF_DH).transpose(3, 0, 2, 1, 4), pos)
    kh = partial_rope(k.reshape(B, L, DIFF_HEADS, 2, DIFF_DH).transpose(3, 0, 2, 1, 4), pos)
    vh = v.reshape(B, L, DIFF_HEADS, DIFF_DV).transpose(0, 2, 1, 3)
    lam = (jnp.exp(jnp.sum(lq1.astype(jnp.float32) * lk1.astype(jnp.float32)))
           - jnp.exp(jnp.sum(lq2.astype(jnp.float32) * lk2.astype(jnp.float32))) + lam_init)
    n_blk = -(-L // Q_BLOCK)
    Lq = n_blk * Q_BLOCK
    qp = jnp.pad(qh, ((0, 0), (0, 0), (0, 0), (0, Lq - L), (0, 0)))
    qb = qp.reshape(2, B, DIFF_HEADS, n_blk, Q_BLOCK, DIFF_DH).transpose(3, 0, 1, 2, 4, 5)
    scale = DIFF_DH ** -0.5

    def block(qblk):
        s = jnp.einsum('ibhqd,ibhkd->ibhqk', qblk, kh, preferred_element_type=jnp.float32) * scale
        p = jax.nn.softmax(s, axis=-1)
        w = p[0] - lam * p[1]
        return jnp.einsum('bhqk,bhke->bhqe', w.astype(vh.dtype), vh)

    o = lax.map(block, qb)
    o = o.transpose(1, 0, 3, 2, 4).reshape(B, Lq, DIFF_HEADS, DIFF_DV)[:, :L]
    o = rmsnorm(o, g_norm) * (1.0 - lam_init)
    return o.reshape(B, L, DIFF_V).astype(v.dtype)


def peer(h, w_q, sub_keys, u_tab, v_tab):
    B, L, D = h.shape
    T = B * L
    TB = PEER_TOKEN_BLOCK
    n_blk = -(-T // TB)
    xp = jnp.pad(h.reshape(T, D), ((0, n_blk * TB - T), (0, 0))).reshape(n_blk, TB, D)
    K = PEER_TOPK

    def block(xb):
        qb = (xb @ w_q).reshape(TB, PEER_HEADS, 2, PEER_DKEY)
        s = jnp.einsum('thpc,hpnc->thpn', qb, sub_keys).astype(jnp.float32)
        sv, si = lax.top_k(s, K)
        cand = (sv[:, :, 0, :, None] + sv[:, :, 1, None, :]).reshape(TB, PEER_HEADS, K * K)
        cidx = (si[:, :, 0, :, None] * PEER_NKEYS + si[:, :, 1, None, :]).reshape(TB, PEER_HEADS, K * K)
        cv, ci = lax.top_k(cand, K)
        eidx = jnp.take_along_axis(cidx, ci, axis=-1)
        g = jax.nn.softmax(cv, axis=-1)
        u = u_tab[eidx]
        vv = v_tab[eidx]
        a = jax.nn.gelu(jnp.einsum('thkd,td->thk', u, xb).astype(jnp.float32), approximate=False)
        return jnp.einsum('thk,thkd->td', (g * a).astype(vv.dtype), vv)

    out = lax.map(block, xp).reshape(n_blk * TB, D)[:T]
    return out.reshape(B, L, D)


def setup_inputs(seed: int = 0) -> dict:
    key = jax.random.key(seed)
    ks = jax.random.split(key, 24)
    nrm = lambda k, shape, s: jax.random.normal(k, shape, jnp.float32) * s
    gain = lambda k, shape: 1.0 + 0.02 * jax.random.normal(k, shape, jnp.float32)
    P = DEPTH
    return {
        'x': nrm(ks[0], (BATCH, SEQ, D_MODEL), 1.0),
        'meta_tokens': nrm(ks[1], (N_META, D_MODEL), 1.0),
        'g_mix': gain(ks[2], (P, D_MODEL)),
        'w_in': nrm(ks[3], (P, D_MODEL, IN_WIDTH), D_MODEL ** -0.5),
        'gla_w2_fwd': nrm(ks[4], (P, GLA_LOWRANK, GLA_QK), GLA_LOWRANK ** -0.5),
        'gla_b_fwd': nrm(ks[5], (P, GLA_QK), 0.1),
        'gla_w2_bwd': nrm(ks[6], (P, GLA_LOWRANK, GLA_QK), GLA_LOWRANK ** -0.5),
        'gla_b_bwd': nrm(ks[7], (P, GLA_QK), 0.1),
        'gla_g_norm': gain(ks[8], (P, GLA_DV)),
        'diff_lq1': nrm(ks[9], (P, DIFF_DH), 0.1),
        'diff_lk1': nrm(ks[10], (P, DIFF_DH), 0.1),
        'diff_lq2': nrm(ks[11], (P, DIFF_DH), 0.1),
        'diff_lk2': nrm(ks[12], (P, DIFF_DH), 0.1),
        'diff_g_norm': gain(ks[13], (P, DIFF_DV)),
        'w_branch_gla': nrm(ks[14], (P, GLA_V, D_MODEL), GLA_V ** -0.5),
        'w_branch_diff': nrm(ks[15], (P, DIFF_V, D_MODEL), DIFF_V ** -0.5),
        'w_out': nrm(ks[16], (P, D_MODEL, D_MODEL), D_MODEL ** -0.5),
        'g_ffn': gain(ks[17], (P, D_MODEL)),
        'peer_w_q': nrm(ks[18], (P, D_MODEL, PEER_HEADS * 2 * PEER_DKEY), D_MODEL ** -0.5),
        'peer_sub_keys': nrm(ks[19], (P, PEER_HEADS, 2, PEER_NKEYS, PEER_DKEY), PEER_DKEY ** -0.5),
        'peer_u': nrm(ks[20], (P, PEER_N, D_MODEL), D_MODEL ** -0.5),
        'peer_v': nrm(ks[21], (P, PEER_N, D_MODEL), 0.25),
        'g_final': gain(ks[22], (D_MODEL,)),
    }


def reference(x, meta_tokens, g_mix, w_in, gla_w2_fwd, gla_b_fwd, gla_w2_bwd, gla_b_bwd, gla_g_norm,
              diff_lq1, diff_lk1, diff_lq2, diff_lk2, diff_g_norm, w_branch_gla, w_branch_diff, w_out,
              g_ffn, peer_w_q, peer_sub_keys, peer_u, peer_v, g_final):
    B = x.shape[0]
    meta = jnp.broadcast_to(meta_tokens[None].astype(x.dtype), (B, N_META, D_MODEL))
    hs = jnp.concatenate([meta, x], axis=1)
    L = hs.shape[1]
    pos = jnp.arange(L, dtype=jnp.int32)
    pts = split_points(IN_SIZES)
    for l in range(DEPTH):
        lam_init = 0.8 - 0.6 * math.exp(-0.3 * l)
        h = rmsnorm(hs, g_mix[l])
        gq, gk, gv, gr, glr, dq, dk, dv, za, zb = jnp.split(h @ w_in[l], pts, axis=-1)
        o_gla = gla_mixer(gq, gk, gv, gr, glr, gla_w2_fwd[l], gla_b_fwd[l], gla_w2_bwd[l], gla_b_bwd[l],
                          gla_g_norm[l])
        o_diff = diff_attention(dq, dk, dv, pos, diff_lq1[l], diff_lk1[l], diff_lq2[l], diff_lk2[l],
                                diff_g_norm[l], lam_init)
        y = jax.nn.sigmoid(za) * (o_gla @ w_branch_gla[l]) + jax.nn.sigmoid(zb) * (o_diff @ w_branch_diff[l])
        hs = hs + y @ w_out[l]
        hs = hs + peer(rmsnorm(hs, g_ffn[l]), peer_w_q[l], peer_sub_keys[l], peer_u[l], peer_v[l])
    out = rmsnorm(hs, g_final)
    return out[:, N_META:]
```

```python
import math
from contextlib import ExitStack

import numpy as np
import concourse.bass as bass
import concourse.mybir as mybir
from concourse.bass_utils import run_bass_kernel_spmd

F32 = mybir.dt.float32
BF16 = mybir.dt.bfloat16
U32 = mybir.dt.uint32
AF = mybir.ActivationFunctionType
ALU = mybir.AluOpType
AX = mybir.AxisListType

ENGS = ("pe", "act", "dve", "pool", "sp")

D = 2048
NB = 18
S = NB * 128
NOWN = 8
TOK = 1024
EPS = 1e-6
IN_W = 16416
OFF_GQ, OFF_GK, OFF_GV, OFF_GR, OFF_LR = 0, 1024, 2048, 4096, 6144
OFF_DQ, OFF_DK, OFF_DV, OFF_ZA, OFF_ZB = 6176, 8224, 10272, 12320, 14368
LAM_INIT = 0.8 - 0.6 * math.exp(-0.3 * 0)
NEXP = 16384
NCH = 128
GRP = 4


class Prog:
    def __init__(self, nc, n_dma_sems=16, same_engine_sync=True):
        self.nc = nc
        self.ops = {e: [] for e in ENGS}
        self.cnt = {e: 0 for e in ENGS}
        self.sem = {e: nc.alloc_semaphore(name=f"sem_{e}") for e in ENGS}
        self.seen = {e: {} for e in ENGS}
        self.last_w = {}
        self.readers = {}
        self.same = same_engine_sync
        self.dsem = {}
        self.dcnt = {}
        self.dnext = {}
        for q in ("sp", "pool", "act"):
            self.dsem[q] = [nc.alloc_semaphore(name=f"dsem_{q}{i}") for i in range(n_dma_sems)]
            self.dcnt[q] = [0] * n_dma_sems
            self.dnext[q] = 0
        self.out_dma = []

    def _semobj(self, key):
        return self.sem[key] if isinstance(key, str) else self.dsem[key[0]][key[1]]

    def _wait(self, eng, key, val):
        if self.seen[eng].get(key, 0) >= val:
            return
        self.seen[eng][key] = val
        s = self._semobj(key)
        self.ops[eng].append(lambda E, s=s, val=val: E.wait_ge(s, val))

    def _deps(self, eng, reads, writes):
        deps = []
        for r in reads:
            if r in self.last_w:
                deps.append(self.last_w[r])
        for w in writes:
            if w in self.last_w:
                deps.append(self.last_w[w])
            deps.extend(self.readers.get(w, ()))
        for (key, val) in deps:
            if key == eng and (eng == "pe" or not self.same):
                continue
            self._wait(eng, key, val)

    def _mark(self, token, reads, writes):
        for w in writes:
            self.last_w[w] = token
            self.readers[w] = []
        for r in reads:
            lst = self.readers.setdefault(r, [])
            lst[:] = [t for t in lst if t[0] != token[0]]
            lst.append(token)

    def op(self, eng, fn, reads=(), writes=()):
        self._deps(eng, reads, writes)
        self.cnt[eng] += 1
        n = self.cnt[eng]
        s = self.sem[eng]
        self.ops[eng].append(lambda E, fn=fn, s=s: fn(E).then_inc(s, 1))
        self._mark((eng, n), reads, writes)

    def dma(self, eng, out, in_, reads=(), writes=(), is_output=False, **kw):
        i = self.dnext[eng]
        self.dnext[eng] = (i + 1) % len(self.dsem[eng])
        key = (eng, i)
        if self.dcnt[eng][i] > 0:
            self._wait(eng, key, self.dcnt[eng][i])
        self._deps(eng, reads, writes)
        self.dcnt[eng][i] += 16
        val = self.dcnt[eng][i]
        s = self.dsem[eng][i]
        self.ops[eng].append(
            lambda E, s=s, out=out, in_=in_, kw=kw: E.dma_start(out=out, in_=in_, **kw).then_inc(s, 16))
        self._mark((key, val), reads, writes)
        if is_output:
            self.out_dma.append((key, val))

    def barrier(self):
        for e in ENGS:
            for f in ENGS:
                if f != e and self.cnt[f] > 0:
                    self._wait(e, f, self.cnt[f])
            for q in self.dsem:
                for i, c in enumerate(self.dcnt[q]):
                    if c > 0:
                        self._wait(e, (q, i), c)

    def finish(self, eng="sp"):
        for (k, v) in self.out_dma:
            self._wait(eng, k, v)
        for e in ENGS:
            if e != eng and self.cnt[e] > 0:
                self._wait(eng, e, self.cnt[e])
        for q in self.dsem:
            for i, c in enumerate(self.dcnt[q]):
                if c > 0:
                    self._wait(eng, (q, i), c)

    def emit(self):
        nc = self.nc
        with nc.Block() as block:
            @block.tensor
            def _(E):
                for f in self.ops["pe"]:
                    f(E)

            @block.scalar
            def _(E):
                for f in self.ops["act"]:
                    f(E)

            @block.vector
            def _(E):
                for f in self.ops["dve"]:
                    f(E)

            @block.gpsimd
            def _(E):
                for f in self.ops["pool"]:
                    f(E)

            @block.sync
            def _(E):
                for f in self.ops["sp"]:
                    f(E)


ALL_INPUTS = []
DBG_SCR = [None]
LIVE = []


class Ring:
    def __init__(self, tiles, name):
        self.tiles = tiles
        self.name = name
        self.i = 0

    def next(self):
        k = self.i % len(self.tiles)
        self.i += 1
        return self.tiles[k], (self.name, k)


def build(stop_after=None, dbg=(), small_tables=False):
    nc = bass.Bass("TRN2", target_bir_lowering=False)
    P = Prog(nc)

    ALL_INPUTS.clear()

    def din(name, shape, dt=F32):
        ap = nc.dram_tensor(name, shape, dt, kind="ExternalInput").ap()
        ALL_INPUTS.append(ap)
        return ap

    def dout(name, shape, dt=F32):
        return nc.dram_tensor(name, shape, dt, kind="ExternalOutput").ap()

    xs = din("xs", [S, D])
    w_in = din("w_in", [D, IN_W])
    w_lr = din("w_lr", [D, 32])
    w2xy = din("w2xy", [2, 17, 1024])
    gvec = din("gvec", [3, D])
    gla_gn = din("gla_gn", [1, 512])
    diff_gn = din("diff_gn", [1, 256])
    lamv = din("lamv", [4, 128])
    tri = din("tri", [4, 128, 128])
    ind2 = din("ind2", [128, 2])
    masks = din("masks", [2, 128, 64])
    cs = din("cs", [2, 128, NB, 16])
    valid = din("valid", [128, NB])
    w_a = din("w_a", [D, D])
    w_b = din("w_b", [D, D])
    w_o = din("w_o", [D, D])
    w_q = din("w_q", [D, D])
    subk = din("subk", [16, 128, 128])
    pu = din("pu", [128 if small_tables else NEXP, D])
    pv = din("pv", [128 if small_tables else NEXP, D])
    out = dout("out", [TOK, D])
    dbg_t = {}
    if "ogT" in dbg:
        dbg_t["ogT"] = dout("dbg_ogT", [D, TOK], BF16)
        dbg_t["odT"] = dout("dbg_odT", [D, TOK], BF16)
    if "hs1" in dbg:
        dbg_t["hs1"] = dout("dbg_hs1", [TOK, D])

    ogT_d = dbg_t["ogT"] if "ogT" in dbg else dout("scr_ogT", [D, TOK], BF16)
    odT_d = dbg_t["odT"] if "ogT" in dbg else dout("scr_odT", [D, TOK], BF16)
    hs1_d = dbg_t["hs1"] if "hs1" in dbg else dout("scr_hs1", [TOK, D], F32)
    wd_d = dout("dbg_wd", [NCH, 128, TOK], BF16) if "wd" in dbg else dout("scr_wd", [NCH, 128, TOK], BF16)

    top = ExitStack()

    class Phase(ExitStack):
        def __exit__(self, *a):
            import os
            LIVE[:] = [x for x in LIVE if x[0] != id(self)]
            if "nobar" not in os.environ.get("DBG_DIFF", ""):
                P.barrier()
            return super().__exit__(*a)

    LIVE.clear()

    def sb(stack, name, shape, dt=F32):
        t = stack.enter_context(nc.sbuf_tensor(name, shape, dt))
        LIVE.append((id(stack), t, False))
        return t

    def pst(stack, name, shape, dt=F32):
        t = stack.enter_context(nc.psum_tensor(name, shape, dt))
        LIVE.append((id(stack), t, True))
        return t

    BK = [pst(top, f"bank{i}", [128, 512], F32) for i in range(8)]

    def bf(i):
        return BK[i][:].bitcast(BF16).rearrange("p (a b) -> p a b", a=8)

    def f3(i, a):
        return BK[i][:].rearrange("p (a b) -> p a b", a=a)

    DBG_SCR[0] = sb(top, "dbg_touch", [1, 64], F32)
    ident_bf = sb(top, "ident_bf", [128, 128], BF16)
    ident_f = sb(top, "ident_f", [128, 128], F32)
    iota_f = sb(top, "iota_f", [128, 128], F32)
    pidx = sb(top, "pidx", [128, 1], F32)
    P.op("pool", lambda E: E.iota(iota_f[:], [[1, 128]], base=0, channel_multiplier=0,
                                  allow_small_or_imprecise_dtypes=True), writes=["iota_f"])
    P.op("pool", lambda E: E.iota(pidx[:], [[0, 1]], base=0, channel_multiplier=1,
                                  allow_small_or_imprecise_dtypes=True), writes=["pidx"])
    P.op("dve", lambda E: E.tensor_scalar(ident_f[:], iota_f[:], pidx[:, 0:1], None, ALU.is_equal),
         reads=["iota_f", "pidx"], writes=["ident_f"])
    P.op("dve", lambda E: E.tensor_scalar(ident_bf[:], iota_f[:], pidx[:, 0:1], None, ALU.is_equal),
         reads=["iota_f", "pidx"], writes=["ident_bf"])

    def norm_rows(x_ap, xkey, g_t, gkey, out_ap, okey, junk, tmp, par):
        ss, rt, rstd = tmp
        k = f"nrm{par}"
        xkeys = xkey if isinstance(xkey, list) else [xkey]
        P.op("act", lambda E: E.activation(junk[:], x_ap, AF.Square, accum_out=ss[:, par:par + 1]),
             reads=xkeys, writes=["junk", k + "ss"])
        P.op("act", lambda E: E.activation(rt[:, par:par + 1], ss[:, par:par + 1], AF.Sqrt,
                                           scale=1.0 / D, bias=EPS),
             reads=[k + "ss"], writes=[k + "rt"])
        P.op("dve", lambda E: E.reciprocal(rstd[:, par:par + 1], rt[:, par:par + 1]),
             reads=[k + "rt"], writes=[k + "rstd"])
        P.op("dve", lambda E: E.scalar_tensor_tensor(out_ap, x_ap, rstd[:, par:par + 1], g_t[:],
                                                     ALU.mult, ALU.mult),
             reads=xkeys + [k + "rstd", gkey], writes=[okey])

    att = ExitStack()
    hT_own = sb(att, "hT_own", [128, 16, TOK], BF16)
    oth = ExitStack()
    hT_oth = sb(oth, "hT_oth", [128, 16, 1280], BF16)

    def hT_blk(blk):
        if blk == 0:
            return hT_oth, 0
        if blk <= NOWN:
            return hT_own, (blk - 1) * 128
        return hT_oth, 128 + (blk - 9) * 128

    ring_tiles = [sb(oth, f"wring{i}", [128, 16, 256], BF16) for i in range(4)]
    ring = Ring(ring_tiles, "wring")

    def load_w(src2d, ncols=256, ring_=None):
        r = ring_ or ring
        t, key = r.next()
        P.dma("pool", t[:, :, 0:ncols], src2d.rearrange("(c p) n -> p c n", p=128), writes=[key])
        return t, key

    with Phase() as ph:
        gm = sb(ph, "gm_bc", [128, D], F32)
        P.dma("sp", gm[:], gvec[0:1, :].partition_broadcast(128), writes=["gm"])
        xt = [sb(ph, f"xt{i}", [128, D], F32) for i in range(2)]
        hb = [sb(ph, f"hb{i}", [128, D], BF16) for i in range(2)]
        junk = sb(ph, "junk0", [128, D], BF16)
        tmp = (sb(ph, "ss0", [128, 2]), sb(ph, "rt0", [128, 2]), sb(ph, "rstd0", [128, 2]))
        tp = [bf(0), bf(1)]
        for blk in range(NB):
            i = blk % 2
            P.dma("sp", xt[i][:], xs[blk * 128:(blk + 1) * 128, :], writes=[("xt", i)])
            norm_rows(xt[i][:], ("xt", i), gm, "gm", hb[i][:], ("hb", i), junk, tmp, i)
            ht, c0 = hT_blk(blk)
            for h8 in range(2):
                for j in range(8):
                    c = h8 * 8 + j
                    P.op("pe", lambda E, h8=h8, j=j, c=c, i=i: E.transpose(
                        tp[h8][:, j, :], hb[i][:, c * 128:(c + 1) * 128], ident_bf[:]),
                        reads=[("hb", i), "ident_bf"], writes=[("tp", h8)])
                eng = "dve" if h8 == 0 else "act"
                if eng == "dve":
                    P.op("dve", lambda E, h8=h8, ht=ht, c0=c0: E.tensor_copy(
                        ht[:, h8 * 8:(h8 + 1) * 8, c0:c0 + 128], tp[h8][:]),
                        reads=[("tp", h8)], writes=[("hT", blk)])
                else:
                    P.op("act", lambda E, h8=h8, ht=ht, c0=c0: E.activation(
                        ht[:, h8 * 8:(h8 + 1) * 8, c0:c0 + 128], tp[h8][:], AF.Copy),
                        reads=[("tp", h8)], writes=[("hT", blk)])
    if stop_after == "ph0":
        hT_dbg = dout("dbg_hT", [128, 16, TOK], BF16)
        P.dma("sp", hT_dbg, hT_own[:], reads=[("hT", b) for b in range(1, 9)], is_output=True)
        return finish_dbg(nc, P, out, [top, att, oth])

    tabs = ExitStack()
    cos_t = sb(tabs, "cos_t", [128, NB, 16])
    sin_t = sb(tabs, "sin_t", [128, NB, 16])
    P.dma("sp", cos_t[:], cs[0], writes=["cos_t"])
    P.dma("sp", sin_t[:], cs[1], writes=["sin_t"])
    valid_t = sb(tabs, "valid_t", [128, NB])
    P.dma("sp", valid_t[:], valid, writes=["valid_t"])
    tri_t = sb(tabs, "tri_t", [128, 4, 128])
    P.dma("sp", tri_t[:], tri.rearrange("a s t -> s a t"), writes=["tri_t"])
    ind2_t = sb(tabs, "ind2_t", [128, 2])
    P.dma("sp", ind2_t[:], ind2, writes=["ind2_t"])
    mask_t = sb(tabs, "mask_t", [128, 2, 64])
    P.dma("sp", mask_t[:], masks.rearrange("a s t -> s a t"), writes=["mask_t"])
    glagn = sb(tabs, "glagn", [128, 512])
    P.dma("sp", glagn[:], gla_gn.partition_broadcast(128), writes=["glagn"])
    diffgn = sb(tabs, "diffgn", [128, 256])
    P.dma("sp", diffgn[:], diff_gn.partition_broadcast(128), writes=["diffgn0"])
    P.op("dve", lambda E: E.tensor_scalar(diffgn[:], diffgn[:], 1.0 - LAM_INIT, None, ALU.mult),
         reads=["diffgn0"], writes=["diffgn"])
    lam_in = sb(tabs, "lam_in", [128, 4, 128])
    for i in range(4):
        P.dma("sp", lam_in[:, i, :], lamv[i:i + 1, :].partition_broadcast(128), writes=[("lam_in", i)])
    lam_tmp = sb(tabs, "lam_tmp", [128, 128])
    lam_s = sb(tabs, "lam_s", [128, 4])
    neglam = sb(tabs, "neglam", [128, 1])
    for i in range(2):
        P.op("dve", lambda E, i=i: E.tensor_tensor(lam_tmp[:], lam_in[:, 2 * i, :], lam_in[:, 2 * i + 1, :], ALU.mult),
             reads=[("lam_in", 2 * i), ("lam_in", 2 * i + 1)], writes=["lam_tmp"])
        P.op("dve", lambda E, i=i: E.tensor_reduce(lam_s[:, i:i + 1], lam_tmp[:], AX.X, ALU.add),
             reads=["lam_tmp"], writes=[("lam_s", i)])
        P.op("act", lambda E, i=i: E.activation(lam_s[:, 2 + i:3 + i], lam_s[:, i:i + 1], AF.Exp),
             reads=[("lam_s", i)], writes=[("lam_e", i)])
    P.op("dve", lambda E: E.tensor_tensor(neglam[:], lam_s[:, 3:4], lam_s[:, 2:3], ALU.subtract),
         reads=[("lam_e", 0), ("lam_e", 1)], writes=["neglam0"])
    P.op("dve", lambda E: E.tensor_scalar(neglam[:], neglam[:], -LAM_INIT, None, ALU.add),
         reads=["neglam0"], writes=["neglam"])

    lr = sb(tabs, "lr", [64, S], BF16)
    w2a = sb(tabs, "w2a", [64, 1024], BF16)
    P.op("pool", lambda E: E.memset(lr[:], 1.0), writes=["lr"])
    P.op("pool", lambda E: E.memset(w2a[:], 0.0), writes=["w2a"])
    P.dma("pool", w2a[0:17, :], w2xy[0], reads=["w2a"], writes=["w2a0"])
    P.dma("pool", w2a[32:49, :], w2xy[1], reads=["w2a"], writes=["w2a1"])
    with Phase() as ph:
        wlr_t = sb(ph, "wlr_t", [128, 16, 32], BF16)
        P.dma("pool", wlr_t[:], w_lr.rearrange("(c p) n -> p c n", p=128), writes=["wlr_t"])
        pl = [BK[0], BK[1]]
        groups = [(hT_oth, 0, 128, 0, [0]), (hT_own, 0, 512, 128, [1, 2, 3, 4]), (hT_own, 512, 512, 640, [5, 6, 7, 8]),
                  (hT_oth, 128, 512, 1152, [9, 10, 11, 12]), (hT_oth, 640, 512, 1664, [13, 14, 15, 16]),
                  (hT_oth, 1152, 128, 2176, [17])]
        for gi, (ht, c0, n, s0, blks) in enumerate(groups):
            for d in range(2):
                for c in range(16):
                    P.op("pe", lambda E, d=d, c=c, ht=ht, c0=c0, n=n: E.matmul(
                        pl[d][d * 32:d * 32 + 16, 0:n], wlr_t[:, c, d * 16:(d + 1) * 16], ht[:, c, c0:c0 + n],
                        start=(c == 0), stop=(c == 15)),
                        reads=["wlr_t"] + [("hT", b) for b in blks], writes=[("pl", d)])
                P.op("dve", lambda E, d=d, n=n, s0=s0: E.tensor_copy(
                    lr[d * 32:d * 32 + 16, s0:s0 + n], pl[d][d * 32:d * 32 + 16, 0:n]),
                    reads=[("pl", d), "lr"], writes=[("lrw", d, gi)])
    lr_keys = [[("lrw", d, gi) for gi in range(6)] for d in range(2)]

    SCALE_Q = 1.0 / 16.0
    with Phase() as ph:
        k_h = sb(ph, "k_h", [128, NB, 256], BF16)
        v_h = sb(ph, "v_h", [128, NB, 512], BF16)
        q_h = sb(ph, "q_h", [128, NOWN, 256], BF16)
        o_acc = sb(ph, "o_acc", [128, NOWN, 512], F32)
        lf = [sb(ph, f"lf{i}", [128, 256]) for i in range(2)]
        ex1 = [sb(ph, f"ex1_{i}", [128, 256]) for i in range(2)]
        Einv = [sb(ph, f"Einv{i}", [128, 256]) for i in range(2)]
        Eq = [sb(ph, f"Eq{i}", [128, 256]) for i in range(2)]
        Est = [sb(ph, f"Est{i}", [128, 256]) for i in range(2)]
        k_in = [sb(ph, f"k_in{i}", [128, 256], BF16) for i in range(2)]
        q_in = [sb(ph, f"q_in{i}", [128, 256], BF16) for i in range(2)]
        k_st = [sb(ph, f"k_st{i}", [128, 256], BF16) for i in range(2)]
        kqT = [sb(ph, f"kqT{i}", [128, 4, 128], BF16) for i in range(2)]
        decay = [sb(ph, f"decay{i}", [128, 2, 2]) for i in range(2)]
        at_sb = [sb(ph, f"at_sb{i}", [128, 64], BF16) for i in range(2)]
        Sst = sb(ph, "Sst", [128, 2, 512], F32)
        Sbf = sb(ph, "Sbf", [128, 2, 512], BF16)
        o_fin = sb(ph, "o_fin", [128, 512], F32)
        junkg = sb(ph, "junkg", [128, 512], BF16)
        sr = sb(ph, "sr", [128, 512], F32)
        ogb = sb(ph, "ogb", [128, 512], BF16)
        ogT_st = [sb(ph, f"ogT_st{i}", [128, 4, 128], BF16) for i in range(2)]
        gss = sb(ph, "gss", [128, 2])
        grt = sb(ph, "grt", [128, 2])
        grs = sb(ph, "grs", [128, 2])
        pj = [BK[0], BK[1]]
        ptr = bf(2)
        pdec = BK[3]
        po = [BK[4], BK[5]]
        pds = [BK[6], BK[7]]
        pjc = [0]

        def pj_next():
            i = pjc[0] % 2
            pjc[0] += 1
            return pj[i], ("pj", i)

        for hd in range(4):
            wq, wqk_ = load_w(w_in[:, OFF_GQ + hd * 256:OFF_GQ + (hd + 1) * 256])
            wk, wkk_ = load_w(w_in[:, OFF_GK + hd * 256:OFF_GK + (hd + 1) * 256])
            wv0, wv0k = load_w(w_in[:, OFF_GV + hd * 512:OFF_GV + hd * 512 + 256])
            wv1, wv1k = load_w(w_in[:, OFF_GV + hd * 512 + 256:OFF_GV + (hd + 1) * 512])
            for blk in range(NB):
                ht, c0 = hT_blk(blk)
                own = 1 <= blk <= NOWN
                pt, pk = pj_next()
                for c in range(16):
                    P.op("pe", lambda E, pt=pt, ht=ht, c0=c0, c=c, wk=wk: E.matmul(
                        pt[:, 0:256], ht[:, c, c0:c0 + 128], wk[:, c, :], start=(c == 0), stop=(c == 15)),
                        reads=[("hT", blk), wkk_], writes=[pk])
                if own:
                    for c in range(16):
                        P.op("pe", lambda E, pt=pt, ht=ht, c0=c0, c=c, wq=wq: E.matmul(
                            pt[:, 256:512], ht[:, c, c0:c0 + 128], wq[:, c, :], start=(c == 0), stop=(c == 15)),
                            reads=[("hT", blk), wqk_], writes=[pk])
                P.op("act", lambda E, pt=pt, blk=blk: E.activation(k_h[:, blk, :], pt[:, 0:256], AF.Copy),
                     reads=[pk], writes=[("k_h", blk)])
                if own:
                    P.op("act", lambda E, pt=pt, blk=blk: E.activation(
                        q_h[:, blk - 1, :], pt[:, 256:512], AF.Copy, scale=SCALE_Q),
                        reads=[pk], writes=[("q_h", blk)])
                pt, pk = pj_next()
                for half_, (wv, wvk) in enumerate(((wv0, wv0k), (wv1, wv1k))):
                    for c in range(16):
                        P.op("pe", lambda E, pt=pt, ht=ht, c0=c0, c=c, wv=wv, half_=half_: E.matmul(
                            pt[:, half_ * 256:(half_ + 1) * 256], ht[:, c, c0:c0 + 128], wv[:, c, :],
                            start=(c == 0), stop=(c == 15)),
                            reads=[("hT", blk), wvk], writes=[pk])
                P.op("dve", lambda E, pt=pt, blk=blk: E.tensor_copy(v_h[:, blk, :], pt[:]),
                     reads=[pk], writes=[("v_h", blk)])
            wr0, wr0k = load_w(w_in[:, OFF_GR + hd * 512:OFF_GR + hd * 512 + 256])
            wr1, wr1k = load_w(w_in[:, OFF_GR + hd * 512 + 256:OFF_GR + (hd + 1) * 512])

            for d in range(2):
                if d == 0:
                    border = [0] + list(range(1, NOWN + 1))
                else:
                    border = list(range(NB - 1, 0, -1))
                first_state = True
                for bi, blk in enumerate(border):
                    own = 1 <= blk <= NOWN
                    par = bi % 2
                    kk = ("gp", par)
                    if d == 0:
                        chunks = [1] if blk == 0 else [0, 1]
                    else:
                        chunks = [0] if blk == NB - 1 else [1, 0]
                    pt, pk = pj_next()
                    P.op("pe", lambda E, pt=pt, d=d, blk=blk, hd=hd: E.matmul(
                        pt[:, 0:256], lr[d * 32:d * 32 + 32, blk * 128:(blk + 1) * 128],
                        w2a[d * 32:d * 32 + 32, hd * 256:(hd + 1) * 256], start=True, stop=True),
                        reads=lr_keys[d] + ["w2a0", "w2a1"], writes=[pk])
                    P.op("act", lambda E, pt=pt, par=par: E.activation(ex1[par][:], pt[:, 0:256], AF.Exp, scale=-1.0),
                         reads=[pk], writes=[("ex1", par)])
                    P.op("act", lambda E, par=par: E.activation(lf[par][:], ex1[par][:], AF.Ln, bias=1.0),
                         reads=[("ex1", par)], writes=[("lf", par)])
                    P.op("pe", lambda E, pt=pt, d=d, par=par: E.matmul(
                        pt[:, 256:512], tri_t[:, 2 + d, :], lf[par][:], start=True, stop=True),
                        reads=[("lf", par), "tri_t"], writes=[pk])
                    P.op("act", lambda E, pt=pt, par=par: E.activation(
                        Est[par][:], pt[:, 256:512], AF.Exp, scale=-1.0 / 16.0),
                        reads=[pk], writes=[("Est", par)])
                    P.op("dve", lambda E, par=par, blk=blk: E.tensor_tensor(
                        k_st[par][:], k_h[:, blk, :], Est[par][:], ALU.mult),
                        reads=[("k_h", blk), ("Est", par)], writes=[("k_st", par)])
                    for dc in range(2):
                        P.op("pe", lambda E, dc=dc, par=par: E.matmul(
                            pdec[:, dc * 2:dc * 2 + 2], lf[par][:, dc * 128:(dc + 1) * 128], ind2_t[:],
                            start=True, stop=True),
                            reads=[("lf", par), "ind2_t"], writes=["pdec_d"])
                    P.op("act", lambda E, par=par: E.activation(
                        decay[par][:].rearrange("p a b -> p (a b)"), pdec[:, 0:4], AF.Exp, scale=-1.0 / 16.0),
                        reads=["pdec_d"], writes=[("decay", par)])
                    if own:
                        pt2, pk2 = pj_next()
                        P.op("pe", lambda E, pt2=pt2, d=d, par=par: E.matmul(
                            pt2[:, 0:256], tri_t[:, d, :], lf[par][:], start=True, stop=True),
                            reads=[("lf", par), "tri_t"], writes=[pk2])
                        P.op("act", lambda E, pt2=pt2, par=par: E.activation(
                            Einv[par][:], pt2[:, 0:256], AF.Exp, scale=1.0 / 16.0),
                            reads=[pk2], writes=[("Einv", par)])
                        P.op("act", lambda E, pt2=pt2, par=par: E.activation(
                            Eq[par][:], pt2[:, 0:256], AF.Exp, scale=-1.0 / 16.0),
                            reads=[pk2], writes=[("Eq", par)])
                        P.op("dve", lambda E, par=par, blk=blk: E.tensor_tensor(
                            k_in[par][:], k_h[:, blk, :], Einv[par][:], ALU.mult),
                            reads=[("k_h", blk), ("Einv", par)], writes=[("k_in", par)])
                        P.op("dve", lambda E, par=par, blk=blk: E.tensor_tensor(
                            q_in[par][:], q_h[:, blk - 1, :], Eq[par][:], ALU.mult),
                            reads=[("q_h", blk), ("Eq", par)], writes=[("q_in", par)])
                        for j in range(2):
                            P.op("pe", lambda E, j=j, par=par: E.transpose(
                                ptr[:, j, :], k_in[par][:, j * 128:(j + 1) * 128], ident_bf[:]),
                                reads=[("k_in", par), "ident_bf"], writes=["ptr"])
                        for j in range(2):
                            P.op("pe", lambda E, j=j, par=par: E.transpose(
                                ptr[:, 2 + j, :], q_in[par][:, j * 128:(j + 1) * 128], ident_bf[:]),
                                reads=[("q_in", par), "ident_bf"], writes=["ptr"])
                        P.op("dve", lambda E, par=par: E.tensor_copy(kqT[par][:], ptr[:, 0:4, :]),
                             reads=["ptr"], writes=[("kqT", par)])
                    pob, pobk = po[par], ("po", par)
                    for cc in chunks:
                        sl = slice(cc * 64, cc * 64 + 64)
                        if own:
                            for dc in range(2):
                                P.op("pe", lambda E, dc=dc, sl=sl, par=par: E.matmul(
                                    pdec[sl, 64:128], kqT[par][:, dc, sl], kqT[par][:, 2 + dc, sl],
                                    start=(dc == 0), stop=(dc == 1)),
                                    reads=[("kqT", par)], writes=[("pdec_a", cc)])
                            P.op("dve", lambda E, sl=sl, par=par, d=d: E.tensor_tensor(
                                at_sb[par][sl, :], pdec[sl, 64:128], mask_t[sl, d, :], ALU.mult),
                                reads=[("pdec_a", cc), "mask_t"], writes=[("at_sb", par, cc)])
                            P.op("pe", lambda E, sl=sl, par=par, blk=blk, pob=pob, fs=first_state: E.matmul(
                                pob[sl, :], at_sb[par][sl, :], v_h[sl, blk, :], start=True, stop=fs),
                                reads=[("at_sb", par, cc), ("v_h", blk)], writes=[(pobk, cc)])
                            if not first_state:
                                for dc in range(2):
                                    P.op("pe", lambda E, sl=sl, par=par, dc=dc, pob=pob: E.matmul(
                                        pob[sl, :], kqT[par][:, 2 + dc, sl], Sbf[:, dc, :],
                                        start=False, stop=(dc == 1)),
                                        reads=[("kqT", par), "Sbf"], writes=[(pobk, cc)])
                        for dc in range(2):
                            P.op("pe", lambda E, sl=sl, par=par, dc=dc, blk=blk: E.matmul(
                                pds[dc][:], k_st[par][sl, dc * 128:(dc + 1) * 128], v_h[sl, blk, :],
                                start=True, stop=True),
                                reads=[("k_st", par), ("v_h", blk)], writes=[("pds", dc)])
                            if first_state:
                                P.op("dve", lambda E, dc=dc: E.tensor_copy(Sst[:, dc, :], pds[dc][:]),
                                     reads=[("pds", dc)], writes=[("Sst", dc)])
                            else:
                                P.op("dve", lambda E, dc=dc, par=par, cc=cc: E.scalar_tensor_tensor(
                                    Sst[:, dc, :], Sst[:, dc, :], decay[par][:, dc, cc:cc + 1], pds[dc][:],
                                    ALU.mult, ALU.add),
                                    reads=[("pds", dc), ("Sst", dc), ("decay", par)], writes=[("Sst", dc)])
                        P.op("act", lambda E: E.activation(Sbf[:], Sst[:], AF.Copy),
                             reads=[("Sst", 0), ("Sst", 1)], writes=["Sbf"])
                        first_state = False
                    if own:
                        ob = blk - 1
                        okeys = [(pobk, 0), (pobk, 1)]
                        if d == 0:
                            P.op("act", lambda E, ob=ob, pob=pob: E.activation(o_acc[:, ob, :], pob[:], AF.Copy),
                                 reads=okeys, writes=[("o_acc", ob)])
                        else:
                            P.op("dve", lambda E, ob=ob, pob=pob: E.tensor_tensor(
                                o_fin[:], pob[:], o_acc[:, ob, :], ALU.add),
                                reads=okeys + [("o_acc", ob)], writes=["o_fin"])
                            P.op("act", lambda E, par=par: E.activation(
                                junkg[:], o_fin[:], AF.Square, accum_out=gss[:, par:par + 1]),
                                reads=["o_fin"], writes=["junkg", ("gss", par)])
                            P.op("act", lambda E, par=par: E.activation(
                                grt[:, par:par + 1], gss[:, par:par + 1], AF.Sqrt, scale=1.0 / 512.0, bias=EPS),
                                reads=[("gss", par)], writes=[("grt", par)])
                            P.op("dve", lambda E, par=par: E.reciprocal(grs[:, par:par + 1], grt[:, par:par + 1]),
                                 reads=[("grt", par)], writes=[("grs", par)])
                            ht, c0 = hT_blk(blk)
                            pt, pk = pj_next()
                            for half_, (wr, wrk) in enumerate(((wr0, wr0k), (wr1, wr1k))):
                                for c in range(16):
                                    P.op("pe", lambda E, pt=pt, ht=ht, c0=c0, c=c, wr=wr, half_=half_: E.matmul(
                                        pt[:, half_ * 256:(half_ + 1) * 256], ht[:, c, c0:c0 + 128], wr[:, c, :],
                                        start=(c == 0), stop=(c == 15)),
                                        reads=[("hT", blk), wrk], writes=[pk])
                            P.op("act", lambda E, pt=pt: E.activation(sr[:], pt[:], AF.Silu),
                                 reads=[pk], writes=["sr"])
                            P.op("dve", lambda E, par=par: E.scalar_tensor_tensor(
                                o_fin[:], o_fin[:], grs[:, par:par + 1], glagn[:], ALU.mult, ALU.mult),
                                reads=["o_fin", ("grs", par), "glagn"], writes=["o_fin"])
                            P.op("dve", lambda E: E.tensor_tensor(ogb[:], o_fin[:], sr[:], ALU.mult),
                                 reads=["o_fin", "sr"], writes=["ogb"])
                            for j in range(4):
                                P.op("pe", lambda E, j=j: E.transpose(
                                    ptr[:, 4 + j, :], ogb[:, j * 128:(j + 1) * 128], ident_bf[:]),
                                    reads=["ogb", "ident_bf"], writes=["ptr2"])
                            P.op("act", lambda E, par=par: E.activation(ogT_st[par][:], ptr[:, 4:8, :], AF.Copy),
                                 reads=["ptr2"], writes=[("ogT_st", par)])
                            P.dma("sp", ogT_d[hd * 512:(hd + 1) * 512, ob * 128:(ob + 1) * 128].rearrange(
                                "(j p) t -> p j t", p=128), ogT_st[par][:],
                                reads=[("ogT_st", par)], writes=[("ogT_d", hd, ob)])
    if stop_after == "gla":
        return finish_dbg(nc, P, out, [tabs, oth, att, top])

    SC = 1.0 / math.sqrt(128.0)
    with Phase() as ph:
        kT_h = sb(ph, "kT_h", [128, 2, S], BF16)
        qT_h = sb(ph, "qT_h", [128, 2, TOK], BF16)
        V_h = sb(ph, "V_h", [128, NB, 258], BF16)
        if stop_after == "dstart0":
            return finish_dbg(nc, P, out, [])
        import os
        if "noop" in os.environ.get("DBG_DIFF", ""):
            pass
        elif "novalid2" in os.environ.get("DBG_DIFF", ""):
            P.op("pool", lambda E: E.memset(V_h[:], 1.0), writes=["V_valid0", "V_valid1"])
        elif "novalid3" in os.environ.get("DBG_DIFF", ""):
            P.op("dve", lambda E: E.memset(kT_h[:, 0, 0:64], 1.0), writes=["V_valid0", "V_valid1"])
        elif "novalid" in os.environ.get("DBG_DIFF", ""):
            P.op("dve", lambda E: E.memset(V_h[:], 1.0), writes=["V_valid0", "V_valid1"])
        else:
            P.op("dve", lambda E: E.tensor_copy(V_h[:, :, 256], valid_t[:]), reads=["valid_t"], writes=["V_valid0"])
            P.op("dve", lambda E: E.tensor_copy(V_h[:, :, 257], valid_t[:]), reads=["valid_t"], writes=["V_valid1"])
        ktm = [sb(ph, f"ktm{i}", [128, 2, 128], BF16) for i in range(2)]
        qtm = [sb(ph, f"qtm{i}", [128, 2, 128], BF16) for i in range(2)]
        rtmp = [sb(ph, f"rtmp{i}", [128, 2, 4, 16]) for i in range(2)]
        rstage = [sb(ph, f"rstage{i}", [128, 256]) for i in range(2)]
        cs2_t = sb(ph, "cs2_t", [128, NB, 64])
        P.op("dve", lambda E: E.tensor_copy(cs2_t[:, :, 0:16], cos_t[:]), reads=["cos_t"], writes=[("cs2", 0)])
        P.op("dve", lambda E: E.tensor_copy(cs2_t[:, :, 16:32], cos_t[:]), reads=["cos_t"], writes=[("cs2", 1)])
        P.op("dve", lambda E: E.tensor_scalar(cs2_t[:, :, 32:48], sin_t[:], -1.0, None, ALU.mult), reads=["sin_t"], writes=[("cs2", 2)])
        P.op("dve", lambda E: E.tensor_copy(cs2_t[:, :, 48:64], sin_t[:]), reads=["sin_t"], writes=["cs2_t"])
        PT = [sb(ph, f"PT{i}", [128, 2, 256], BF16) for i in range(3)]
        rz = sb(ph, "rz", [128, 4])
        t1 = sb(ph, "t1", [128, 256])
        od = sb(ph, "od", [128, 256])
        junkd = sb(ph, "junkd", [128, 256], BF16)
        dss = sb(ph, "dss", [128, 2])
        drt = sb(ph, "drt", [128, 2])
        drs = sb(ph, "drs", [128, 2])
        odb = sb(ph, "odb", [128, 256], BF16)
        odT_st = [sb(ph, f"odT_st{i}", [128, 2, 128], BF16) for i in range(2)]
        if stop_after == "dstart1":
            return finish_dbg(nc, P, out, [])
        pst2_ = [BK[0], BK[1]]
        pst_ = [t[:].rearrange("p (m q) -> p m q", m=2) for t in pst2_]
        ptr = bf(2)
        pO = [BK[3], BK[4], BK[5], BK[6]]
        pjc = [0]

        def pj_next():
            i = pjc[0] % 2
            pjc[0] += 1
            return pst2_[i][:], ("ast", i)

        import os
        FLAGS = os.environ.get("DBG_DIFF", "")

        def rope(dst, src_ps, blk, par, key_in, key_out):
            if "norope" in FLAGS:
                P.op("act", lambda E: E.activation(dst[:].rearrange("p m d -> p (m d)"), src_ps, AF.Copy),
                     reads=[key_in], writes=[(key_out, "c")])
                return [(key_out, "c")]
            return rope_(dst, src_ps, blk, par, key_in, key_out)

        def rope_(dst, src_ps, blk, par, key_in, key_out):
            r = rtmp[par]
            st = rstage[par]
            rk = ("rtmp", par)
            sk = ("rstage", par)
            P.op("act", lambda E: E.activation(st[:], src_ps, AF.Copy), reads=[key_in], writes=[sk])
            P.op("act", lambda E: E.activation(dst[:].rearrange("p m d -> p (m d)"), st[:], AF.Copy),
                 reads=[sk], writes=[(key_out, "c")])
            keys = [(key_out, "c")]
            c2 = cs2_t[:, blk, 0:32]
            s2 = cs2_t[:, blk, 32:64]
            for m in range(2):
                rr = r[:, m].rearrange("p a b -> p (a b)")
                x = st[:, m * 128:m * 128 + 32]
                P.op("dve", lambda E, rr=rr, m=m: E.tensor_copy(rr[:, 0:16], st[:, m * 128 + 16:m * 128 + 32]),
                     reads=[sk], writes=[(rk, m, 0)])
                P.op("dve", lambda E, rr=rr, m=m: E.tensor_copy(rr[:, 16:32], st[:, m * 128:m * 128 + 16]),
                     reads=[sk], writes=[(rk, m, 1)])
                P.op("dve", lambda E, rr=rr: E.tensor_tensor(rr[:, 0:32], rr[:, 0:32], s2, ALU.mult),
                     reads=[(rk, m, 0), (rk, m, 1), "cs2_t", ("cs2", 0), ("cs2", 1), ("cs2", 2)], writes=[(rk, m, 2)])
                P.op("dve", lambda E, rr=rr, x=x: E.tensor_tensor(rr[:, 32:64], x, c2, ALU.mult),
                     reads=[sk, "cs2_t", ("cs2", 0), ("cs2", 1), ("cs2", 2)], writes=[(rk, m, 3)])
                P.op("dve", lambda E, rr=rr, m=m: E.tensor_tensor(dst[:, m, 0:32], rr[:, 0:32], rr[:, 32:64], ALU.add),
                     reads=[(rk, m, 2), (rk, m, 3), (key_out, "c")], writes=[(key_out, "a", m)])
                keys += [(key_out, "a", m)]
            return keys

        if stop_after == "dstart":
            return finish_dbg(nc, P, out, [])
        for h in range(8):
            wq, wqk_ = load_w(w_in[:, OFF_DQ + h * 256:OFF_DQ + (h + 1) * 256])
            wk, wkk_ = load_w(w_in[:, OFF_DK + h * 256:OFF_DK + (h + 1) * 256])
            wv, wvk_ = load_w(w_in[:, OFF_DV + h * 256:OFF_DV + (h + 1) * 256])
            for blk in range(NB):
                ht, c0 = hT_blk(blk)
                own = 1 <= blk <= NOWN
                par = blk % 2
                pt, pk = pj_next()
                for c in range(16):
                    P.op("pe", lambda E, pt=pt, ht=ht, c0=c0, c=c, wk=wk: E.matmul(
                        pt[:, 0:256], ht[:, c, c0:c0 + 128], wk[:, c, :], start=(c == 0), stop=(c == 15)),
                        reads=[("hT", blk), wkk_], writes=[pk])
                for c in range(16):
                    P.op("pe", lambda E, pt=pt, ht=ht, c0=c0, c=c, wv=wv: E.matmul(
                        pt[:, 256:512], ht[:, c, c0:c0 + 128], wv[:, c, :], start=(c == 0), stop=(c == 15)),
                        reads=[("hT", blk), wvk_], writes=[pk])
                P.op("act", lambda E, pt=pt, blk=blk: E.activation(V_h[:, blk, 0:256], pt[:, 256:512], AF.Copy),
                     reads=[pk], writes=[("V_h", blk)])
                kkeys = rope(ktm[par], pt[:, 0:256], blk, par, pk, ("ktm", par))
                for m in range(2):
                    P.op("pe", lambda E, m=m, par=par: E.transpose(ptr[:, m, :], ktm[par][:, m, :], ident_bf[:]),
                         reads=kkeys + ["ident_bf"], writes=["aptr_k"])
                P.op("act", lambda E, blk=blk: E.activation(
                    kT_h[:, :, blk * 128:(blk + 1) * 128], ptr[:, 0:2, :], AF.Copy),
                    reads=["aptr_k"], writes=[("kT_h", blk)])
                if own:
                    pt, pk = pj_next()
                    for c in range(16):
                        P.op("pe", lambda E, pt=pt, ht=ht, c0=c0, c=c, wq=wq: E.matmul(
                            pt[:, 0:256], ht[:, c, c0:c0 + 128], wq[:, c, :], start=(c == 0), stop=(c == 15)),
                            reads=[("hT", blk), wqk_], writes=[pk])
                    qkeys = rope(qtm[par], pt[:, 0:256], blk, par, pk, ("qtm", par))
                    for m in range(2):
                        P.op("pe", lambda E, m=m, par=par: E.transpose(ptr[:, 2 + m, :], qtm[par][:, m, :], ident_bf[:]),
                             reads=qkeys + ["ident_bf"], writes=["aptr_q"])
                    P.op("dve", lambda E, blk=blk: E.tensor_copy(
                        qT_h[:, :, (blk - 1) * 128:blk * 128], ptr[:, 2:4, :]),
                        reads=["aptr_q"], writes=[("qT_h", blk - 1)])
            if stop_after == "dproj":
                d1 = dout("dbg_kT", [128, 2, S], BF16)
                d2 = dout("dbg_qT", [128, 2, TOK], BF16)
                d3 = dout("dbg_V", [128, NB, 258], BF16)
                P.dma("sp", d1, kT_h[:], reads=[("kT_h", b) for b in range(NB)], is_output=True)
                P.dma("sp", d2, qT_h[:], reads=[("qT_h", b) for b in range(NOWN)], is_output=True)
                P.dma("sp", d3, V_h[:], reads=[("V_h", b) for b in range(NB)] + ["V_valid0", "V_valid1"], is_output=True)
                return finish_dbg(nc, P, out, [])
            pti = 0
            for qg in range(4):
                for kb in range(NB):
                    stp = kb % 2
                    for m in range(2):
                        P.op("pe", lambda E, m=m, stp=stp, kb=kb, qg=qg: E.matmul(
                            pst_[stp][:, m, :], kT_h[:, m, kb * 128:(kb + 1) * 128],
                            qT_h[:, m, qg * 256:(qg + 1) * 256], start=True, stop=True),
                            reads=[("kT_h", kb), ("qT_h", 2 * qg), ("qT_h", 2 * qg + 1)], writes=[("ast", stp)])
                    pp = pti % 3
                    pti += 1
                    P.op("act", lambda E, pp=pp, stp=stp: E.activation(PT[pp][:], pst_[stp], AF.Exp, scale=SC),
                         reads=[("ast", stp)], writes=[("PT", pp)])
                    for j in range(2):
                        for m in range(2):
                            P.op("pe", lambda E, j=j, m=m, pp=pp, kb=kb: E.matmul(
                                pO[j * 2 + m][:, 0:258], PT[pp][:, m, j * 128:(j + 1) * 128], V_h[:, kb, :],
                                start=(kb == 0), stop=(kb == NB - 1)),
                                reads=[("PT", pp), ("V_h", kb), "V_valid0", "V_valid1"], writes=[("aO", j, m)])
                for j in range(2):
                    qb = qg * 2 + j
                    par = j
                    P.op("dve", lambda E, j=j: E.reciprocal(rz[:, j * 2:j * 2 + 1], pO[j * 2][:, 256:257]),
                         reads=[("aO", j, 0)], writes=[("rz", j, 0)])
                    P.op("dve", lambda E, j=j: E.reciprocal(rz[:, j * 2 + 1:j * 2 + 2], pO[j * 2 + 1][:, 256:257]),
                         reads=[("aO", j, 1)], writes=[("rz", j, 1)])
                    P.op("dve", lambda E, j=j: E.tensor_scalar(
                        t1[:], pO[j * 2 + 1][:, 0:256], rz[:, j * 2 + 1:j * 2 + 2], neglam[:, 0:1], ALU.mult, ALU.mult),
                        reads=[("aO", j, 1), ("rz", j, 1), "neglam"], writes=["t1"])
                    P.op("dve", lambda E, j=j: E.scalar_tensor_tensor(
                        od[:], pO[j * 2][:, 0:256], rz[:, j * 2:j * 2 + 1], t1[:], ALU.mult, ALU.add),
                        reads=[("aO", j, 0), ("rz", j, 0), "t1"], writes=["od"])
                    P.op("act", lambda E, par=par: E.activation(
                        junkd[:], od[:], AF.Square, accum_out=dss[:, par:par + 1]),
                        reads=["od"], writes=["junkd", ("dss", par)])
                    P.op("act", lambda E, par=par: E.activation(
                        drt[:, par:par + 1], dss[:, par:par + 1], AF.Sqrt, scale=1.0 / 256.0, bias=EPS),
                        reads=[("dss", par)], writes=[("drt", par)])
                    P.op("dve", lambda E, par=par: E.reciprocal(drs[:, par:par + 1], drt[:, par:par + 1]),
                         reads=[("drt", par)], writes=[("drs", par)])
                    P.op("dve", lambda E, par=par: E.scalar_tensor_tensor(
                        odb[:], od[:], drs[:, par:par + 1], diffgn[:], ALU.mult, ALU.mult),
                        reads=["od", ("drs", par), "diffgn"], writes=["odb"])
                    for i2 in range(2):
                        P.op("pe", lambda E, i2=i2: E.transpose(
                            ptr[:, 4 + i2, :], odb[:, i2 * 128:(i2 + 1) * 128], ident_bf[:]),
                            reads=["odb", "ident_bf"], writes=["aptr_o"])
                    P.op("act", lambda E, par=par: E.activation(odT_st[par][:], ptr[:, 4:6, :], AF.Copy),
                         reads=["aptr_o"], writes=[("odT_st", par)])
                    P.dma("sp", odT_d[h * 256:(h + 1) * 256, qb * 128:(qb + 1) * 128].rearrange(
                        "(j p) t -> p j t", p=128), odT_st[par][:],
                        reads=[("odT_st", par)], writes=[("odT_d", h, qb)])
    if stop_after == "attn":
        return finish_dbg(nc, P, out, [tabs, oth, att, top])
    P.barrier()
    tabs.close()
    oth.close()

    with Phase() as ph:
        yT = sb(ph, "yT", [128, 16, TOK], BF16)
        with Phase() as ph2:
            ring2 = Ring([sb(ph2, f"wringD{i}", [128, 16, 256], BF16) for i in range(4)], "wringD")
            og_g = sb(ph2, "og_g", [128, 16, TOK], BF16)
            od_g = sb(ph2, "od_g", [128, 16, TOK], BF16)
            P.dma("sp", og_g[:], ogT_d.rearrange("(c p) t -> p c t", p=128),
                  reads=[("ogT_d", hd, ob) for hd in range(4) for ob in range(8)], writes=["og_g"])
            P.dma("sp", od_g[:], odT_d.rearrange("(c p) t -> p c t", p=128),
                  reads=[("odT_d", h, qb) for h in range(8) for qb in range(8)], writes=["od_g"])
            sa = [sb(ph2, f"sa{i}", [128, 512]) for i in range(2)]
            sbb = [sb(ph2, f"sbb{i}", [128, 512]) for i in range(2)]
            ta = [sb(ph2, f"ta{i}", [128, 512]) for i in range(2)]
            tb_ = [sb(ph2, f"tb{i}", [128, 512]) for i in range(2)]
            pU = BK
            own_keys = [("hT", b) for b in range(1, 9)]
            it = 0
            for f2 in range(8):
                wa_t, wa_k = load_w(w_a[:, f2 * 256:(f2 + 1) * 256], ring_=ring2)
                wb_t, wb_k = load_w(w_b[:, f2 * 256:(f2 + 1) * 256], ring_=ring2)
                wza_t, wza_k = load_w(w_in[:, OFF_ZA + f2 * 256:OFF_ZA + (f2 + 1) * 256], ring_=ring2)
                wzb_t, wzb_k = load_w(w_in[:, OFF_ZB + f2 * 256:OFF_ZB + (f2 + 1) * 256], ring_=ring2)
                for j in range(2):
                    fc = f2 * 2 + j
                    for tg in range(2):
                        par = it % 2
                        it += 1
                        banks = pU[par * 4:(par + 1) * 4]
                        bk = [("dU", par, q) for q in range(4)]
                        srcs = [(wa_t, wa_k, og_g, ["og_g"]), (wb_t, wb_k, od_g, ["od_g"]),
                                (wza_t, wza_k, hT_own, own_keys), (wzb_t, wzb_k, hT_own, own_keys)]
                        for q, (wt, wk_, act_t, akeys) in enumerate(srcs):
                            for c in range(16):
                                P.op("pe", lambda E, q=q, wt=wt, act_t=act_t, c=c, j=j, tg=tg, banks=banks: E.matmul(
                                    banks[q][:], wt[:, c, j * 128:(j + 1) * 128], act_t[:, c, tg * 512:(tg + 1) * 512],
                                    start=(c == 0), stop=(c == 15)),
                                    reads=[wk_] + akeys, writes=[bk[q]])
                        P.op("act", lambda E, par=par, banks=banks: E.activation(sa[par][:], banks[2][:], AF.Sigmoid),
                             reads=[bk[2]], writes=[("sa", par)])
                        P.op("act", lambda E, par=par, banks=banks: E.activation(sbb[par][:], banks[3][:], AF.Sigmoid),
                             reads=[bk[3]], writes=[("sbb", par)])
                        P.op("dve", lambda E, par=par, banks=banks: E.tensor_tensor(ta[par][:], banks[0][:], sa[par][:], ALU.mult),
                             reads=[bk[0], ("sa", par)], writes=[("ta", par)])
                        P.op("dve", lambda E, par=par, banks=banks: E.tensor_tensor(tb_[par][:], banks[1][:], sbb[par][:], ALU.mult),
                             reads=[bk[1], ("sbb", par)], writes=[("tb", par)])
                        P.op("dve", lambda E, par=par, fc=fc, tg=tg: E.tensor_tensor(
                            yT[:, fc, tg * 512:(tg + 1) * 512], ta[par][:], tb_[par][:], ALU.add),
                            reads=[("ta", par), ("tb", par)], writes=[("yT", fc, tg)])
        with Phase() as ph2:
            ring3 = Ring([sb(ph2, f"wringO{i}", [128, 16, 256], BF16) for i in range(3)], "wringO")
            xres = [sb(ph2, f"xres{i}", [128, 256]) for i in range(3)]
            hsb = [sb(ph2, f"hsb{i}", [128, 256]) for i in range(3)]
            pH = [BK[0], BK[1]]
            ykeys = [("yT", fc, tg) for fc in range(16) for tg in range(2)]
            it = 0
            for nt in range(8):
                wo_t, wo_k = load_w(w_o[:, nt * 256:(nt + 1) * 256], ring_=ring3)
                for ob in range(NOWN):
                    par = it % 2
                    p3 = it % 3
                    it += 1
                    P.dma("sp", xres[p3][:], xs[(ob + 1) * 128:(ob + 2) * 128, nt * 256:(nt + 1) * 256],
                          writes=[("xres", p3)])
                    for c in range(16):
                        P.op("pe", lambda E, par=par, c=c, ob=ob, wo_t=wo_t: E.matmul(
                            pH[par][:, 0:256], yT[:, c, ob * 128:(ob + 1) * 128], wo_t[:, c, :],
                            start=(c == 0), stop=(c == 15)),
                            reads=[wo_k] + ykeys, writes=[("dH", par)])
                    P.op("dve", lambda E, par=par, p3=p3: E.tensor_tensor(
                        hsb[p3][:], pH[par][:, 0:256], xres[p3][:], ALU.add),
                        reads=[("dH", par), ("xres", p3)], writes=[("hsb", p3)])
                    P.dma("sp", hs1_d[ob * 128:(ob + 1) * 128, nt * 256:(nt + 1) * 256], hsb[p3][:],
                          reads=[("hsb", p3)], writes=[("hs1_d", ob, nt)])
    att.close()
    if stop_after == "hs1":
        return finish_dbg(nc, P, out, [top])

    hs1_keys = [("hs1_d", ob, nt) for ob in range(8) for nt in range(8)]

    peer = ExitStack()
    xnT = sb(peer, "xnT", [128, 16, TOK], BF16)
    with Phase() as ph:
        gm1 = sb(ph, "gf_bc", [128, D], F32)
        P.dma("sp", gm1[:], gvec[1:2, :].partition_broadcast(128), writes=["gf"])
        xt1 = [sb(ph, f"pxt{i}", [128, D], F32) for i in range(2)]
        hb1 = [sb(ph, f"phb{i}", [128, D], BF16) for i in range(2)]
        junk1 = sb(ph, "pjunk", [128, D], BF16)
        tmp1 = (sb(ph, "pss", [128, 2]), sb(ph, "prt", [128, 2]), sb(ph, "prstd", [128, 2]))
        tp1 = [bf(0), bf(1)]
        for ob in range(NOWN):
            i = ob % 2
            P.dma("sp", xt1[i][:], hs1_d[ob * 128:(ob + 1) * 128, :],
                  reads=[("hs1_d", ob, nt) for nt in range(8)], writes=[("pxt", i)])
            norm_rows(xt1[i][:], ("pxt", i), gm1, "gf", hb1[i][:], ("phb", i), junk1, tmp1, i)
            for h8 in range(2):
                for j in range(8):
                    c = h8 * 8 + j
                    P.op("pe", lambda E, h8=h8, j=j, c=c, i=i: E.transpose(
                        tp1[h8][:, j, :], hb1[i][:, c * 128:(c + 1) * 128], ident_bf[:]),
                        reads=[("phb", i), "ident_bf"], writes=[("ptp", h8)])
                if h8 == 0:
                    P.op("dve", lambda E, h8=h8, ob=ob: E.tensor_copy(
                        xnT[:, h8 * 8:(h8 + 1) * 8, ob * 128:(ob + 1) * 128], tp1[h8][:]),
                        reads=[("ptp", h8)], writes=[("xnT", ob)])
                else:
                    P.op("act", lambda E, h8=h8, ob=ob: E.activation(
                        xnT[:, h8 * 8:(h8 + 1) * 8, ob * 128:(ob + 1) * 128], tp1[h8][:], AF.Copy),
                        reads=[("ptp", h8)], writes=[("xnT", ob)])
    xn_keys = [("xnT", ob) for ob in range(NOWN)]

    with Phase() as ph:
        qT = sb(ph, "pqT", [128, 16, TOK], BF16)
        skT = sb(ph, "skT", [128, 16, 128], BF16)
        with Phase() as ph2:
            ring4 = Ring([sb(ph2, f"wringQ{i}", [128, 16, 256], BF16) for i in range(3)], "wringQ")
            skf = sb(ph2, "skf", [128, 16, 128], F32)
            P.dma("sp", skf[:], subk.rearrange("a n c -> n a c"), writes=["skf"])
            pq = [BK[0], BK[1]]
            ptk = f3(2, 4)
            for a4 in range(4):
                for j in range(4):
                    P.op("pe", lambda E, a4=a4, j=j: E.transpose(ptk[:, j, :], skf[:, a4 * 4 + j, :], ident_f[:]),
                         reads=["skf", "ident_f"], writes=["ptk"])
                P.op("dve", lambda E, a4=a4: E.tensor_copy(skT[:, a4 * 4:(a4 + 1) * 4, :], ptk[:]),
                     reads=["ptk"], writes=[("skT", a4)])
            it = 0
            for w2_ in range(8):
                wq_t, wq_k = load_w(w_q[:, w2_ * 256:(w2_ + 1) * 256], ring_=ring4)
                for j in range(2):
                    hp = w2_ * 2 + j
                    for tg in range(2):
                        par = it % 2
                        it += 1
                        for c in range(16):
                            P.op("pe", lambda E, par=par, c=c, j=j, tg=tg, wq_t=wq_t: E.matmul(
                                pq[par][:], wq_t[:, c, j * 128:(j + 1) * 128], xnT[:, c, tg * 512:(tg + 1) * 512],
                                start=(c == 0), stop=(c == 15)),
                                reads=[wq_k] + xn_keys, writes=[("pq", par)])
                        if tg == 0:
                            P.op("act", lambda E, par=par, hp=hp, tg=tg: E.activation(
                                qT[:, hp, tg * 512:(tg + 1) * 512], pq[par][:], AF.Copy),
                                reads=[("pq", par)], writes=[("pqT", hp, tg)])
                        else:
                            P.op("dve", lambda E, par=par, hp=hp, tg=tg: E.tensor_copy(
                                qT[:, hp, tg * 512:(tg + 1) * 512], pq[par][:]),
                                reads=[("pq", par)], writes=[("pqT", hp, tg)])
        skT_keys = [("skT", a4) for a4 in range(4)]
        with Phase() as ph2:
            s_sb = sb(ph2, "s_sb", [128, 16, 128])
            s_w = sb(ph2, "s_w", [128, 16, 128])
            sv = sb(ph2, "sv", [128, 16, 16])
            si = sb(ph2, "si", [128, 16, 16], U32)
            sif = sb(ph2, "sif", [128, 16, 16])
            cand = sb(ph2, "cand", [128, 8, 256])
            cand_w = sb(ph2, "cand_w", [128, 8, 256])
            cv = sb(ph2, "cv", [128, 8, 16])
            ci = sb(ph2, "ci", [128, 8, 16], U32)
            k12u = sb(ph2, "k12u", [128, 2, 8, 16], U32)
            k12f = sb(ph2, "k12f", [128, 2, 8, 16])
            eq = sb(ph2, "eq", [128, 8, 16, 16])
            IJ = sb(ph2, "IJ", [128, 3, 128])
            cvm = sb(ph2, "cvm", [128, 8, 16])
            zs = sb(ph2, "zs", [128, 8])
            rzs = sb(ph2, "rzs", [128, 8])
            IJT = sb(ph2, "IJT", [128, 3, 128])
            NAB = 8
            A_t = [sb(ph2, f"A_t{i}", [128, 128], BF16) for i in range(NAB)]
            B_t = [sb(ph2, f"B_t{i}", [128, 128], BF16) for i in range(NAB)]
            WT = [sb(ph2, f"WT{i}", [128, 128, 128], BF16) for i in range(2)]
            ps_s = [f3(i, 4) for i in range(4)]
            ps_w = [f3(4, 4), f3(5, 4)]
            ps_t = f3(6, 4)[:, 0:3, :]
            iota16 = iota_f[:, 0:16]
            for tb in range(NOWN):
                wpar = tb % 2
                for hp in range(16):
                    P.op("pe", lambda E, hp=hp, tb=tb: E.matmul(
                        ps_s[hp // 4][:, hp % 4, :], qT[:, hp, tb * 128:(tb + 1) * 128], skT[:, hp, :],
                        start=True, stop=True),
                        reads=[("pqT", hp, tb // 4)] + skT_keys, writes=[("ps_s", hp // 4)])
                for b4 in range(4):
                    P.op("act", lambda E, b4=b4: E.activation(s_sb[:, b4 * 4:(b4 + 1) * 4, :], ps_s[b4][:], AF.Copy),
                         reads=[("ps_s", b4)], writes=[("s_sb", b4)])
                for hp in range(16):
                    sk_ = ("s_sb", hp // 4)
                    P.op("dve", lambda E, hp=hp: E.max(sv[:, hp, 0:8], s_sb[:, hp, :]),
                         reads=[sk_], writes=[("sv", hp, 0)])
                    P.op("dve", lambda E, hp=hp: E.max_index(si[:, hp, 0:8], sv[:, hp, 0:8], s_sb[:, hp, :]),
                         reads=[sk_, ("sv", hp, 0)], writes=[("si", hp, 0)])
                    P.op("dve", lambda E, hp=hp: E.match_replace(s_w[:, hp, :], sv[:, hp, 0:8], s_sb[:, hp, :], -1e30),
                         reads=[sk_, ("sv", hp, 0)], writes=[("s_w", hp)])
                    P.op("dve", lambda E, hp=hp: E.max(sv[:, hp, 8:16], s_w[:, hp, :]),
                         reads=[("s_w", hp)], writes=[("sv", hp, 1)])
                    P.op("dve", lambda E, hp=hp: E.max_index(si[:, hp, 8:16], sv[:, hp, 8:16], s_w[:, hp, :]),
                         reads=[("s_w", hp), ("sv", hp, 1)], writes=[("si", hp, 1)])
                svk = [("sv", hp, z) for hp in range(16) for z in range(2)]
                sik = [("si", hp, z) for hp in range(16) for z in range(2)]
                P.op("dve", lambda E: E.tensor_copy(sif[:], si[:]), reads=sik, writes=["sif"])
                sv4 = sv[:].rearrange("p (h two) k -> p h two k", two=2)
                sif4 = sif[:].rearrange("p (h two) k -> p h two k", two=2)
                P.op("dve", lambda E: E.tensor_tensor(
                    cand[:].rearrange("p h (a b) -> p h a b", a=16),
                    sv4[:, :, 0, :].unsqueeze(3).to_broadcast([128, 8, 16, 16]),
                    sv4[:, :, 1, :].unsqueeze(2).to_broadcast([128, 8, 16, 16]), ALU.add),
                    reads=svk, writes=["cand"])
                for h in range(8):
                    P.op("dve", lambda E, h=h: E.max(cv[:, h, 0:8], cand[:, h, :]),
                         reads=["cand"], writes=[("cv", h, 0)])
                    P.op("dve", lambda E, h=h: E.max_index(ci[:, h, 0:8], cv[:, h, 0:8], cand[:, h, :]),
                         reads=["cand", ("cv", h, 0)], writes=[("ci", h, 0)])
                    P.op("dve", lambda E, h=h: E.match_replace(cand_w[:, h, :], cv[:, h, 0:8], cand[:, h, :], -1e30),
                         reads=["cand", ("cv", h, 0)], writes=[("cand_w", h)])
                    P.op("dve", lambda E, h=h: E.max(cv[:, h, 8:16], cand_w[:, h, :]),
                         reads=[("cand_w", h)], writes=[("cv", h, 1)])
                    P.op("dve", lambda E, h=h: E.max_index(ci[:, h, 8:16], cv[:, h, 8:16], cand_w[:, h, :]),
                         reads=[("cand_w", h), ("cv", h, 1)], writes=[("ci", h, 1)])
                cvk = [("cv", h, z) for h in range(8) for z in range(2)]
                cik = [("ci", h, z) for h in range(8) for z in range(2)]
                P.op("dve", lambda E: E.tensor_single_scalar(k12u[:, 0, :, :], ci[:], 4, ALU.logical_shift_right),
                     reads=cik, writes=["k1u"])
                P.op("dve", lambda E: E.tensor_single_scalar(k12u[:, 1, :, :], ci[:], 15, ALU.bitwise_and),
                     reads=cik, writes=["k2u"])
                P.op("dve", lambda E: E.tensor_copy(k12f[:], k12u[:]), reads=["k1u", "k2u"], writes=["k12f"])
                for z in range(2):
                    P.op("dve", lambda E, z=z: E.tensor_tensor(
                        eq[:], k12f[:, z, :, :].unsqueeze(3).to_broadcast([128, 8, 16, 16]),
                        iota16.unsqueeze(1).unsqueeze(1).to_broadcast([128, 8, 16, 16]), ALU.is_equal),
                        reads=["k12f", "iota_f"], writes=["eq"])
                    P.op("dve", lambda E, z=z: E.tensor_tensor(
                        eq[:], eq[:], sif4[:, :, z, :].unsqueeze(2).to_broadcast([128, 8, 16, 16]), ALU.mult),
                        reads=["eq", "sif"], writes=["eq"])
                    P.op("dve", lambda E, z=z: E.tensor_reduce(
                        IJ[:, z, :].rearrange("p (h k) -> p h k", h=8), eq[:], AX.X, ALU.add),
                        reads=["eq"], writes=[("IJ", z)])
                P.op("dve", lambda E: E.tensor_tensor(
                    cvm[:], cv[:], cv[:, :, 0:1].to_broadcast([128, 8, 16]), ALU.subtract),
                    reads=cvk, writes=["cvm"])
                P.op("act", lambda E: E.activation(cvm[:], cvm[:], AF.Exp), reads=["cvm"], writes=["cvm"])
                P.op("dve", lambda E: E.tensor_reduce(zs[:], cvm[:], AX.X, ALU.add), reads=["cvm"], writes=["zs"])
                P.op("dve", lambda E: E.reciprocal(rzs[:], zs[:]), reads=["zs"], writes=["rzs"])
                P.op("dve", lambda E: E.tensor_tensor(
                    IJ[:, 2, :].rearrange("p (h k) -> p h k", h=8), cvm[:],
                    rzs[:].unsqueeze(2).to_broadcast([128, 8, 16]), ALU.mult),
                    reads=["cvm", "rzs"], writes=[("IJ", 2)])
                for z in range(3):
                    P.op("pe", lambda E, z=z: E.transpose(ps_t[:, z, :], IJ[:, z, :], ident_f[:]),
                         reads=[("IJ", z), "ident_f"], writes=["ps_t"])
                P.op("act", lambda E: E.activation(IJT[:], ps_t[:], AF.Copy), reads=["ps_t"], writes=["IJT"])
                for t in range(128):
                    ab = t % NAB
                    P.op("dve", lambda E, t=t, ab=ab: E.tensor_scalar(
                        A_t[ab][:], iota_f[:], IJT[:, 0, t:t + 1], IJT[:, 2, t:t + 1], ALU.is_equal, ALU.mult),
                        reads=["IJT", "iota_f"], writes=[("A_t", ab)])
                    P.op("dve", lambda E, t=t, ab=ab: E.tensor_scalar(
                        B_t[ab][:], iota_f[:], IJT[:, 1, t:t + 1], None, ALU.is_equal),
                        reads=["IJT", "iota_f"], writes=[("B_t", ab)])
                    wb = (t // 4) % 2
                    P.op("pe", lambda E, t=t, ab=ab, wb=wb: E.matmul(
                        ps_w[wb][:, t % 4, :], B_t[ab][:], A_t[ab][:], start=True, stop=True),
                        reads=[("A_t", ab), ("B_t", ab)], writes=[("ps_w", wb)])
                    if t % 4 == 3:
                        t0 = t - 3
                        P.op("act", lambda E, t0=t0, wb=wb, wpar=wpar: E.activation(
                            WT[wpar][:, :, t0:t0 + 4].rearrange("j i t -> j t i"), ps_w[wb][:], AF.Copy),
                            reads=[("ps_w", wb)], writes=[("WT", wpar, t0 // 4)])
                if stop_after == "p3" and tb == NOWN - 1:
                    dIJ = dout("dbg_IJ", [128, 3, 128])
                    dsv = dout("dbg_sv", [128, 16, 16])
                    dxn = dout("dbg_xnT", [128, 16, TOK], BF16)
                    dcv = dout("dbg_cv", [128, 8, 16])
                    P.dma("sp", dIJ, IJ[:], reads=[("IJ", z) for z in range(3)], is_output=True)
                    P.dma("sp", dsv, sv[:], reads=svk, is_output=True)
                    P.dma("sp", dxn, xnT[:], reads=xn_keys, is_output=True)
                    P.dma("sp", dcv, cv[:], reads=cvk, is_output=True)
                for i8 in range(8):
                    P.dma("sp", wd_d[i8 * 16:(i8 + 1) * 16, :, tb * 128:(tb + 1) * 128].rearrange("i j t -> j i t"),
                          WT[wpar][:, i8 * 16:(i8 + 1) * 16, :],
                          reads=[("WT", wpar, q4) for q4 in range(32)], writes=[("wd_d", tb, i8)])
    wd_keys = [("wd_d", tb, i8) for tb in range(8) for i8 in range(8)]
    if stop_after == "p3":
        return finish_dbg(nc, P, out, [])

    with Phase() as ph:
        acc = sb(ph, "acc", [128, NOWN, D], F32)
        P.dma("sp", acc[:], hs1_d.rearrange("(b p) d -> p b d", p=128), reads=hs1_keys, writes=["acc"])
        NU = 2
        u_c = [sb(ph, f"u_c{i}", [128, D], BF16) for i in range(NU)]
        uT_c = [sb(ph, f"uT_c{i}", [128, 16, 128], BF16) for i in range(NU)]
        NV = GRP + 2
        v_c = [sb(ph, f"v_c{i}", [128, D], BF16) for i in range(NV)]
        wt_c = [sb(ph, f"wt_c{i}", [128, TOK], BF16) for i in range(2)]
        ga = [sb(ph, f"ga{i}", [128, TOK], BF16) for i in range(2)]
        NL = 2 * GRP
        lh = [sb(ph, f"lh{i}", [128, TOK], BF16) for i in range(NL)]
        ptp = [bf(0), bf(1)]
        pa = [BK[2], BK[3]]
        pout = [BK[4], BK[5], BK[6], BK[7]]

        def load_chunk(ci_):
            ui = ci_ % NU
            vi = ci_ % NV
            wi = ci_ % 2
            for hh in range(2):
                P.dma("pool", u_c[ui][:, hh * 1024:(hh + 1) * 1024], pu[ci_ * 128:(ci_ + 1) * 128, hh * 1024:(hh + 1) * 1024],
                      writes=[("u_c", ui, hh)])
                P.dma("pool", v_c[vi][:, hh * 1024:(hh + 1) * 1024], pv[ci_ * 128:(ci_ + 1) * 128, hh * 1024:(hh + 1) * 1024],
                      writes=[("v_c", vi, hh)])
            P.dma("sp", wt_c[wi][:], wd_d[ci_], reads=wd_keys, writes=[("wt_c", wi)])

        load_chunk(0)
        for ci_ in range(NCH):
            if ci_ + 1 < NCH:
                load_chunk(ci_ + 1)
            ui = ci_ % NU
            vi = ci_ % NV
            wi = ci_ % 2
            li = ci_ % NL
            gi = ci_ % 2
            for h8 in range(2):
                for j in range(8):
                    c = h8 * 8 + j
                    P.op("pe", lambda E, h8=h8, j=j, c=c, ui=ui: E.transpose(
                        ptp[h8][:, j, :], u_c[ui][:, c * 128:(c + 1) * 128], ident_bf[:]),
                        reads=[("u_c", ui, h8), "ident_bf"], writes=[("qtp", h8)])
                if h8 == 0:
                    P.op("act", lambda E, h8=h8, ui=ui: E.activation(
                        uT_c[ui][:, h8 * 8:(h8 + 1) * 8, :], ptp[h8][:], AF.Copy),
                        reads=[("qtp", h8)], writes=[("uT_c", ui, h8)])
                else:
                    P.op("dve", lambda E, h8=h8, ui=ui: E.tensor_copy(
                        uT_c[ui][:, h8 * 8:(h8 + 1) * 8, :], ptp[h8][:]),
                        reads=[("qtp", h8)], writes=[("uT_c", ui, h8)])
            for tg in range(2):
                for c in range(16):
                    P.op("pe", lambda E, tg=tg, c=c, ui=ui: E.matmul(
                        pa[tg][:], uT_c[ui][:, c, :], xnT[:, c, tg * 512:(tg + 1) * 512],
                        start=(c == 0), stop=(c == 15)),
                        reads=[("uT_c", ui, c // 8)] + xn_keys, writes=[("qa", tg)])
                P.op("act", lambda E, tg=tg, gi=gi: E.activation(
                    ga[gi][:, tg * 512:(tg + 1) * 512], pa[tg][:], AF.Gelu),
                    reads=[("qa", tg)], writes=[("ga", gi, tg)])
            P.op("dve", lambda E, gi=gi, wi=wi, li=li: E.tensor_tensor(lh[li][:], ga[gi][:], wt_c[wi][:], ALU.mult),
                 reads=[("ga", gi, 0), ("ga", gi, 1), ("wt_c", wi)], writes=[("lh", li)])
            if ci_ % GRP == GRP - 1:
                cs_ = list(range(ci_ - GRP + 1, ci_ + 1))
                for tb in range(NOWN):
                    for ng in range(4):
                        for k_, cc in enumerate(cs_):
                            P.op("pe", lambda E, tb=tb, ng=ng, cc=cc, k_=k_: E.matmul(
                                pout[ng][:], lh[cc % NL][:, tb * 128:(tb + 1) * 128],
                                v_c[cc % NV][:, ng * 512:(ng + 1) * 512],
                                start=(k_ == 0), stop=(k_ == GRP - 1)),
                                reads=[("lh", cc % NL), ("v_c", cc % NV, ng // 2)], writes=[("qo", ng)])
                        P.op("dve", lambda E, tb=tb, ng=ng: E.tensor_tensor(
                            acc[:, tb, ng * 512:(ng + 1) * 512], pout[ng][:], acc[:, tb, ng * 512:(ng + 1) * 512],
                            ALU.add),
                            reads=[("qo", ng), "acc", ("acc", tb, ng)], writes=[("acc", tb, ng)])
        gfin = sb(ph, "gfin", [128, D], F32)
        P.dma("sp", gfin[:], gvec[2:3, :].partition_broadcast(128), writes=["gfin"])
        outb = [sb(ph, f"outb{i}", [128, D], F32) for i in range(2)]
        tmpf = (sb(ph, "fss", [128, 2]), sb(ph, "frt", [128, 2]), sb(ph, "frstd", [128, 2]))
        junk2 = sb(ph, "junk2", [128, D], BF16)
        for ob in range(NOWN):
            i = ob % 2
            norm_rows(acc[:, ob, :], [("acc", ob, ng) for ng in range(4)], gfin, "gfin", outb[i][:], ("outb", i), junk2, tmpf, i)
            P.dma("sp", out[ob * 128:(ob + 1) * 128, :], outb[i][:], reads=[("outb", i)], writes=[("out", ob)],
                  is_output=True)
    peer.close()
    top.close()
    P.finish()
    P.emit()
    return nc


def finish_dbg(nc, P, out, stacks):
    scr = DBG_SCR[0]
    P.barrier()
    for k, (sid, t, isps) in enumerate(LIVE):
        ap = t[:]
        while len(ap.shape) > 2:
            ap = ap[:, 0]
        if isps:
            P.op("dve", lambda E, ap=ap: E.tensor_copy(scr[0:1, 32:33], ap[0:1, 0:1]), writes=[("lv", k)])
        else:
            P.op("dve", lambda E, ap=ap: E.tensor_copy(scr[0:1, 32:33], ap[0:1, 0:1]), writes=[("lv", k)])
    for i, ap in enumerate(ALL_INPUTS):
        flat = ap
        while len(flat.shape) > 2:
            flat = flat[0]
        n = min(16, flat.shape[1])
        P.dma("sp", scr[0:1, 0:n], flat[0:1, 0:n], writes=[("touch", i)])
    P.dma("sp", out[0:1, 0:64], scr[0:1, 0:64], reads=[("touch", i) for i in range(len(ALL_INPUTS))], is_output=True)
    P.finish()
    P.emit()
    return nc


def _consts():
    idx = np.arange(128)
    same = (idx[:, None] // 64) == (idx[None, :] // 64)
    s, t = idx[:, None], idx[None, :]
    triX = (same & (s <= t)).astype(np.float32)
    triY = (same & (s >= t)).astype(np.float32)
    aftX = (same & (s > t)).astype(np.float32)
    aftY = (same & (s < t)).astype(np.float32)
    tri = np.stack([triX, triY, aftX, aftY], 0)
    ind2 = np.zeros((128, 2), np.float32)
    ind2[:64, 0] = 1.0
    ind2[64:, 1] = 1.0
    return tri, ind2


def _core_inputs(inp, b, half, shared):
    x = inp["x"][b]
    meta = inp["meta_tokens"]
    xs = np.zeros((S, D), np.float32)
    pos = np.zeros(S, np.float64)
    valid = np.zeros(S, np.float32)
    ar = np.arange(1024)
    if half == 0:
        xs[112:128] = meta
        pos[112:128] = np.arange(16)
        valid[112:128] = 1
        xs[128:1152] = x[0:1024]
        pos[128:1152] = 16 + ar
        xs[1152:2176] = x[1024:2048]
        pos[1152:2176] = 16 + 1024 + ar
        valid[128:2176] = 1
    else:
        xs[128:1152] = x[2047:1023:-1]
        pos[128:1152] = 16 + 2047 - ar
        xs[1152:2176] = x[1023::-1]
        pos[1152:2176] = 16 + 1023 - ar
        xs[2176:2192] = meta[::-1]
        pos[2176:2192] = 15 - np.arange(16)
        valid[128:2192] = 1
    hf = 16
    inv = 1.0 / (500000.0 ** (np.arange(hf, dtype=np.float32) / hf))
    ang = pos.astype(np.float32)[:, None] * inv[None, :].astype(np.float32)
    cos = np.cos(ang).astype(np.float32)
    sin = np.sin(ang).astype(np.float32)
    cs = np.stack([cos.reshape(NB, 128, 16).transpose(1, 0, 2), sin.reshape(NB, 128, 16).transpose(1, 0, 2)], 0)
    w_in = inp["w_in"][0]
    lr_f = w_in[:, OFF_LR:OFF_LR + 16]
    lr_b = w_in[:, OFF_LR + 16:OFF_LR + 32]
    fw = np.concatenate([inp["gla_w2_fwd"][0], inp["gla_b_fwd"][0][None]], 0)
    bw = np.concatenate([inp["gla_w2_bwd"][0], inp["gla_b_bwd"][0][None]], 0)
    idx = np.arange(64)
    s, t = idx[:, None], idx[None, :]
    if half == 0:
        w_lr = np.concatenate([lr_f, lr_b], 1)
        w2xy = np.stack([fw, bw], 0)
        mX = (s <= t)
        mY = (s > t)
    else:
        w_lr = np.concatenate([lr_b, lr_f], 1)
        w2xy = np.stack([bw, fw], 0)
        mX = (s < t)
        mY = (s >= t)
    masks = np.stack([np.concatenate([mX, mX], 0), np.concatenate([mY, mY], 0)], 0).astype(np.float32)
    d = dict(shared)
    d.update({
        "xs": xs,
        "w_lr": np.ascontiguousarray(w_lr),
        "w2xy": np.ascontiguousarray(w2xy),
        "masks": masks,
        "cs": np.ascontiguousarray(cs),
        "valid": np.ascontiguousarray(valid.reshape(NB, 128).T),
    })
    return d


def _shared_inputs(inp):
    tri, ind2 = _consts()
    return {
        "w_in": np.ascontiguousarray(inp["w_in"][0]),
        "gvec": np.ascontiguousarray(np.stack([inp["g_mix"][0], inp["g_ffn"][0], inp["g_final"]], 0)),
        "gla_gn": np.ascontiguousarray(inp["gla_g_norm"][0][None]),
        "diff_gn": np.ascontiguousarray(inp["diff_g_norm"][0][None]),
        "lamv": np.ascontiguousarray(np.stack([inp["diff_lq1"][0], inp["diff_lk1"][0], inp["diff_lq2"][0],
                                               inp["diff_lk2"][0]], 0)),
        "tri": tri,
        "ind2": ind2,
        "w_a": np.ascontiguousarray(inp["w_branch_gla"][0]),
        "w_b": np.ascontiguousarray(inp["w_branch_diff"][0]),
        "w_o": np.ascontiguousarray(inp["w_out"][0]),
        "w_q": np.ascontiguousarray(inp["peer_w_q"][0]),
        "subk": np.ascontiguousarray(inp["peer_sub_keys"][0].reshape(16, 128, 128)),
        "pu": np.ascontiguousarray(inp["peer_u"][0]),
        "pv": np.ascontiguousarray(inp["peer_v"][0]),
    }


def make_in_maps(inputs):
    inp = {k: np.asarray(v, dtype=np.float32) for k, v in inputs.items()}
    shared = _shared_inputs(inp)
    return [_core_inputs(inp, c // 2, c % 2, shared) for c in range(8)]


def assemble(results, key="out"):
    o = np.zeros((4, 2048, D), np.float32)
    for c in range(8):
        b, half = c // 2, c % 2
        r = np.asarray(results[c][key])
        if half == 0:
            o[b, 0:1024] = r
        else:
            o[b, 1024:2048] = r[::-1]
    return o


def kernel(**inputs):
    nc = build()
    in_maps = make_in_maps(inputs)
    res = run_bass_kernel_spmd(nc, in_maps, core_ids=list(range(8)))
    return assemble(res.results)
```

```python
import math
from contextlib import ExitStack

import numpy as np
import concourse.bass as bass
import concourse.mybir as mybir
from concourse.bass_utils import run_bass_kernel_spmd

F32 = mybir.dt.float32
BF16 = mybir.dt.bfloat16
U32 = mybir.dt.uint32
AF = mybir.ActivationFunctionType
ALU = mybir.AluOpType
AX = mybir.AxisListType

ENGS = ("pe", "act", "dve", "pool", "sp")

D = 2048
NB = 18
S = NB * 128
NOWN = 8
TOK = 1024
EPS = 1e-6
IN_W = 16416
OFF_GQ, OFF_GK, OFF_GV, OFF_GR, OFF_LR = 0, 1024, 2048, 4096, 6144
OFF_DQ, OFF_DK, OFF_DV, OFF_ZA, OFF_ZB = 6176, 8224, 10272, 12320, 14368
LAM_INIT = 0.8 - 0.6 * math.exp(-0.3 * 0)
NEXP = 16384
NCH = 128
GRP = 4


class Prog:
    def __init__(self, nc, n_dma_sems=16, same_engine_sync=True):
        self.nc = nc
        self.ops = {e: [] for e in ENGS}
        self.cnt = {e: 0 for e in ENGS}
        self.sem = {e: nc.alloc_semaphore(name=f"sem_{e}") for e in ENGS}
        self.seen = {e: {} for e in ENGS}
        self.last_w = {}
        self.readers = {}
        self.same = same_engine_sync
        self.dsem = {}
        self.dcnt = {}
        self.dnext = {}
        for q in ("sp", "pool", "act"):
            self.dsem[q] = [nc.alloc_semaphore(name=f"dsem_{q}{i}") for i in range(n_dma_sems)]
            self.dcnt[q] = [0] * n_dma_sems
            self.dnext[q] = 0
        self.out_dma = []

    def _semobj(self, key):
        return self.sem[key] if isinstance(key, str) else self.dsem[key[0]][key[1]]

    def _wait(self, eng, key, val):
        if self.seen[eng].get(key, 0) >= val:
            return
        self.seen[eng][key] = val
        s = self._semobj(key)
        self.ops[eng].append(lambda E, s=s, val=val: E.wait_ge(s, val))

    def _deps(self, eng, reads, writes):
        deps = []
        for r in reads:
            if r in self.last_w:
                deps.append(self.last_w[r])
        for w in writes:
            if w in self.last_w:
                deps.append(self.last_w[w])
            deps.extend(self.readers.get(w, ()))
        for (key, val) in deps:
            if key == eng and (eng == "pe" or not self.same):
                continue
            self._wait(eng, key, val)

    def _mark(self, token, reads, writes):
        for w in writes:
            self.last_w[w] = token
            self.readers[w] = []
        for r in reads:
            lst = self.readers.setdefault(r, [])
            lst[:] = [t for t in lst if t[0] != token[0]]
            lst.append(token)

    def op(self, eng, fn, reads=(), writes=()):
        self._deps(eng, reads, writes)
        self.cnt[eng] += 1
        n = self.cnt[eng]
        s = self.sem[eng]
        self.ops[eng].append(lambda E, fn=fn, s=s: fn(E).then_inc(s, 1))
        self._mark((eng, n), reads, writes)

    def dma(self, eng, out, in_, reads=(), writes=(), is_output=False, **kw):
        i = self.dnext[eng]
        self.dnext[eng] = (i + 1) % len(self.dsem[eng])
        key = (eng, i)
        if self.dcnt[eng][i] > 0:
            self._wait(eng, key, self.dcnt[eng][i])
        self._deps(eng, reads, writes)
        self.dcnt[eng][i] += 16
        val = self.dcnt[eng][i]
        s = self.dsem[eng][i]
        self.ops[eng].append(
            lambda E, s=s, out=out, in_=in_, kw=kw: E.dma_start(out=out, in_=in_, **kw).then_inc(s, 16))
        self._mark((key, val), reads, writes)
        if is_output:
            self.out_dma.append((key, val))

    def barrier(self):
        for e in ENGS:
            for f in ENGS:
                if f != e and self.cnt[f] > 0:
                    self._wait(e, f, self.cnt[f])
            for q in self.dsem:
                for i, c in enumerate(self.dcnt[q]):
                    if c > 0:
                        self._wait(e, (q, i), c)

    def finish(self, eng="sp"):
        for (k, v) in self.out_dma:
            self._wait(eng, k, v)
        for e in ENGS:
            if e != eng and self.cnt[e] > 0:
                self._wait(eng, e, self.cnt[e])
        for q in self.dsem:
            for i, c in enumerate(self.dcnt[q]):
                if c > 0:
                    self._wait(eng, (q, i), c)

    def emit(self):
        nc = self.nc
        with nc.Block() as block:
            @block.tensor
            def _(E):
                for f in self.ops["pe"]:
                    f(E)

            @block.scalar
            def _(E):
                for f in self.ops["act"]:
                    f(E)

            @block.vector
            def _(E):
                for f in self.ops["dve"]:
                    f(E)

            @block.gpsimd
            def _(E):
                for f in self.ops["pool"]:
                    f(E)

            @block.sync
            def _(E):
                for f in self.ops["sp"]:
                    f(E)


ALL_INPUTS = []
DBG_SCR = [None]
LIVE = []


class Ring:
    def __init__(self, tiles, name):
        self.tiles = tiles
        self.name = name
        self.i = 0

    def next(self):
        k = self.i % len(self.tiles)
        self.i += 1
        return self.tiles[k], (self.name, k)


def build(stop_after=None, dbg=(), small_tables=False):
    nc = bass.Bass("TRN2", target_bir_lowering=False)
    P = Prog(nc)

    ALL_INPUTS.clear()

    def din(name, shape, dt=F32):
        ap = nc.dram_tensor(name, shape, dt, kind="ExternalInput").ap()
        ALL_INPUTS.append(ap)
        return ap

    def dout(name, shape, dt=F32):
        return nc.dram_tensor(name, shape, dt, kind="ExternalOutput").ap()

    xs = din("xs", [S, D])
    w_in = din("w_in", [D, IN_W])
    w_lr = din("w_lr", [D, 32])
    w2xy = din("w2xy", [2, 17, 1024])
    gvec = din("gvec", [3, D])
    gla_gn = din("gla_gn", [1, 512])
    diff_gn = din("diff_gn", [1, 256])
    lamv = din("lamv", [4, 128])
    tri = din("tri", [4, 128, 128])
    ind2 = din("ind2", [128, 2])
    masks = din("masks", [2, 128, 64])
    cs = din("cs", [2, 128, NB, 16])
    valid = din("valid", [128, NB])
    w_a = din("w_a", [D, D])
    w_b = din("w_b", [D, D])
    w_o = din("w_o", [D, D])
    w_q = din("w_q", [D, D])
    subk = din("subk", [16, 128, 128])
    pu = din("pu", [128 if small_tables else NEXP, D])
    pv = din("pv", [128 if small_tables else NEXP, D])
    out = dout("out", [TOK, D])
    dbg_t = {}
    if "ogT" in dbg:
        dbg_t["ogT"] = dout("dbg_ogT", [D, TOK], BF16)
        dbg_t["odT"] = dout("dbg_odT", [D, TOK], BF16)
    if "hs1" in dbg:
        dbg_t["hs1"] = dout("dbg_hs1", [TOK, D])

    ogT_d = dbg_t["ogT"] if "ogT" in dbg else dout("scr_ogT", [D, TOK], BF16)
    odT_d = dbg_t["odT"] if "ogT" in dbg else dout("scr_odT", [D, TOK], BF16)
    hs1_d = dbg_t["hs1"] if "hs1" in dbg else dout("scr_hs1", [TOK, D], F32)
    wd_d = dout("dbg_wd", [NCH, 128, TOK], BF16) if "wd" in dbg else dout("scr_wd", [NCH, 128, TOK], BF16)

    top = ExitStack()

    class Phase(ExitStack):
        def __exit__(self, *a):
            import os
            LIVE[:] = [x for x in LIVE if x[0] != id(self)]
            if "nobar" not in os.environ.get("DBG_DIFF", ""):
                P.barrier()
            return super().__exit__(*a)

    LIVE.clear()

    def sb(stack, name, shape, dt=F32):
        t = stack.enter_context(nc.sbuf_tensor(name, shape, dt))
        LIVE.append((id(stack), t, False))
        return t

    def pst(stack, name, shape, dt=F32):
        t = stack.enter_context(nc.psum_tensor(name, shape, dt))
        LIVE.append((id(stack), t, True))
        return t

    BK = [pst(top, f"bank{i}", [128, 512], F32) for i in range(8)]

    def bf(i):
        return BK[i][:].bitcast(BF16).rearrange("p (a b) -> p a b", a=8)

    def f3(i, a):
        return BK[i][:].rearrange("p (a b) -> p a b", a=a)

    DBG_SCR[0] = sb(top, "dbg_touch", [1, 64], F32)
    ident_bf = sb(top, "ident_bf", [128, 128], BF16)
    ident_f = sb(top, "ident_f", [128, 128], F32)
    iota_f = sb(top, "iota_f", [128, 128], F32)
    pidx = sb(top, "pidx", [128, 1], F32)
    P.op("pool", lambda E: E.iota(iota_f[:], [[1, 128]], base=0, channel_multiplier=0,
                                  allow_small_or_imprecise_dtypes=True), writes=["iota_f"])
    P.op("pool", lambda E: E.iota(pidx[:], [[0, 1]], base=0, channel_multiplier=1,
                                  allow_small_or_imprecise_dtypes=True), writes=["pidx"])
    P.op("dve", lambda E: E.tensor_scalar(ident_f[:], iota_f[:], pidx[:, 0:1], None, ALU.is_equal),
         reads=["iota_f", "pidx"], writes=["ident_f"])
    P.op("dve", lambda E: E.tensor_scalar(ident_bf[:], iota_f[:], pidx[:, 0:1], None, ALU.is_equal),
         reads=["iota_f", "pidx"], writes=["ident_bf"])

    def norm_rows(x_ap, xkey, g_t, gkey, out_ap, okey, junk, tmp, par):
        ss, rt, rstd = tmp
        k = f"nrm{par}"
        xkeys = xkey if isinstance(xkey, list) else [xkey]
        P.op("act", lambda E: E.activation(junk[:], x_ap, AF.Square, accum_out=ss[:, par:par + 1]),
             reads=xkeys, writes=["junk", k + "ss"])
        P.op("act", lambda E: E.activation(rt[:, par:par + 1], ss[:, par:par + 1], AF.Sqrt,
                                           scale=1.0 / D, bias=EPS),
             reads=[k + "ss"], writes=[k + "rt"])
        P.op("dve", lambda E: E.reciprocal(rstd[:, par:par + 1], rt[:, par:par + 1]),
             reads=[k + "rt"], writes=[k + "rstd"])
        P.op("dve", lambda E: E.scalar_tensor_tensor(out_ap, x_ap, rstd[:, par:par + 1], g_t[:],
                                                     ALU.mult, ALU.mult),
             reads=xkeys + [k + "rstd", gkey], writes=[okey])

    att = ExitStack()
    hT_own = sb(att, "hT_own", [128, 16, TOK], BF16)
    oth = ExitStack()
    hT_oth = sb(oth, "hT_oth", [128, 16, 1280], BF16)

    def hT_blk(blk):
        if blk == 0:
            return hT_oth, 0
        if blk <= NOWN:
            return hT_own, (blk - 1) * 128
        return hT_oth, 128 + (blk - 9) * 128

    ring_tiles = [sb(oth, f"wring{i}", [128, 16, 256], BF16) for i in range(5)]
    ring = Ring(ring_tiles, "wring")

    def load_w(src2d, ncols=256, ring_=None):
        r = ring_ or ring
        t, key = r.next()
        P.dma("pool", t[:, :, 0:ncols], src2d.rearrange("(c p) n -> p c n", p=128), writes=[key])
        return t, key

    with Phase() as ph:
        gm = sb(ph, "gm_bc", [128, D], F32)
        P.dma("sp", gm[:], gvec[0:1, :].partition_broadcast(128), writes=["gm"])
        xt = [sb(ph, f"xt{i}", [128, D], F32) for i in range(2)]
        hb = [sb(ph, f"hb{i}", [128, D], BF16) for i in range(2)]
        junk = sb(ph, "junk0", [128, D], BF16)
        tmp = (sb(ph, "ss0", [128, 2]), sb(ph, "rt0", [128, 2]), sb(ph, "rstd0", [128, 2]))
        tp = [bf(0), bf(1)]
        for blk in range(NB):
            i = blk % 2
            P.dma("sp", xt[i][:], xs[blk * 128:(blk + 1) * 128, :], writes=[("xt", i)])
            norm_rows(xt[i][:], ("xt", i), gm, "gm", hb[i][:], ("hb", i), junk, tmp, i)
            ht, c0 = hT_blk(blk)
            for h8 in range(2):
                for j in range(8):
                    c = h8 * 8 + j
                    P.op("pe", lambda E, h8=h8, j=j, c=c, i=i: E.transpose(
                        tp[h8][:, j, :], hb[i][:, c * 128:(c + 1) * 128], ident_bf[:]),
                        reads=[("hb", i), "ident_bf"], writes=[("tp", h8)])
                eng = "dve" if h8 == 0 else "act"
                if eng == "dve":
                    P.op("dve", lambda E, h8=h8, ht=ht, c0=c0: E.tensor_copy(
                        ht[:, h8 * 8:(h8 + 1) * 8, c0:c0 + 128], tp[h8][:]),
                        reads=[("tp", h8)], writes=[("hT", blk)])
                else:
                    P.op("act", lambda E, h8=h8, ht=ht, c0=c0: E.activation(
                        ht[:, h8 * 8:(h8 + 1) * 8, c0:c0 + 128], tp[h8][:], AF.Copy),
                        reads=[("tp", h8)], writes=[("hT", blk)])
    if stop_after == "ph0":
        hT_dbg = dout("dbg_hT", [128, 16, TOK], BF16)
        P.dma("sp", hT_dbg, hT_own[:], reads=[("hT", b) for b in range(1, 9)], is_output=True)
        return finish_dbg(nc, P, out, [top, att, oth])

    tabs = ExitStack()
    cos_t = sb(tabs, "cos_t", [128, NB, 16])
    sin_t = sb(tabs, "sin_t", [128, NB, 16])
    P.dma("sp", cos_t[:], cs[0], writes=["cos_t"])
    P.dma("sp", sin_t[:], cs[1], writes=["sin_t"])
    valid_t = sb(tabs, "valid_t", [128, NB])
    P.dma("sp", valid_t[:], valid, writes=["valid_t"])
    tri_t = sb(tabs, "tri_t", [128, 4, 128])
    P.dma("sp", tri_t[:], tri.rearrange("a s t -> s a t"), writes=["tri_t"])
    ind2_t = sb(tabs, "ind2_t", [128, 2])
    P.dma("sp", ind2_t[:], ind2, writes=["ind2_t"])
    mask_t = sb(tabs, "mask_t", [128, 2, 64])
    P.dma("sp", mask_t[:], masks.rearrange("a s t -> s a t"), writes=["mask_t"])
    glagn = sb(tabs, "glagn", [128, 512])
    P.dma("sp", glagn[:], gla_gn.partition_broadcast(128), writes=["glagn"])
    diffgn = sb(tabs, "diffgn", [128, 256])
    P.dma("sp", diffgn[:], diff_gn.partition_broadcast(128), writes=["diffgn0"])
    P.op("dve", lambda E: E.tensor_scalar(diffgn[:], diffgn[:], 1.0 - LAM_INIT, None, ALU.mult),
         reads=["diffgn0"], writes=["diffgn"])
    lam_in = sb(tabs, "lam_in", [128, 4, 128])
    for i in range(4):
        P.dma("sp", lam_in[:, i, :], lamv[i:i + 1, :].partition_broadcast(128), writes=[("lam_in", i)])
    lam_tmp = sb(tabs, "lam_tmp", [128, 128])
    lam_s = sb(tabs, "lam_s", [128, 4])
    neglam = sb(tabs, "neglam", [128, 1])
    for i in range(2):
        P.op("dve", lambda E, i=i: E.tensor_tensor(lam_tmp[:], lam_in[:, 2 * i, :], lam_in[:, 2 * i + 1, :], ALU.mult),
             reads=[("lam_in", 2 * i), ("lam_in", 2 * i + 1)], writes=["lam_tmp"])
        P.op("dve", lambda E, i=i: E.tensor_reduce(lam_s[:, i:i + 1], lam_tmp[:], AX.X, ALU.add),
             reads=["lam_tmp"], writes=[("lam_s", i)])
        P.op("act", lambda E, i=i: E.activation(lam_s[:, 2 + i:3 + i], lam_s[:, i:i + 1], AF.Exp),
             reads=[("lam_s", i)], writes=[("lam_e", i)])
    P.op("dve", lambda E: E.tensor_tensor(neglam[:], lam_s[:, 3:4], lam_s[:, 2:3], ALU.subtract),
         reads=[("lam_e", 0), ("lam_e", 1)], writes=["neglam0"])
    P.op("dve", lambda E: E.tensor_scalar(neglam[:], neglam[:], -LAM_INIT, None, ALU.add),
         reads=["neglam0"], writes=["neglam"])

    lr = sb(tabs, "lr", [64, S], BF16)
    w2a = sb(tabs, "w2a", [64, 1024], BF16)
    P.op("pool", lambda E: E.memset(lr[:], 1.0), writes=["lr"])
    P.op("pool", lambda E: E.memset(w2a[:], 0.0), writes=["w2a"])
    P.dma("pool", w2a[0:17, :], w2xy[0], reads=["w2a"], writes=["w2a0"])
    P.dma("pool", w2a[32:49, :], w2xy[1], reads=["w2a"], writes=["w2a1"])
    with Phase() as ph:
        wlr_t = sb(ph, "wlr_t", [128, 16, 32], BF16)
        P.dma("pool", wlr_t[:], w_lr.rearrange("(c p) n -> p c n", p=128), writes=["wlr_t"])
        pl = [BK[0], BK[1]]
        groups = [(hT_oth, 0, 128, 0, [0]), (hT_own, 0, 512, 128, [1, 2, 3, 4]), (hT_own, 512, 512, 640, [5, 6, 7, 8]),
                  (hT_oth, 128, 512, 1152, [9, 10, 11, 12]), (hT_oth, 640, 512, 1664, [13, 14, 15, 16]),
                  (hT_oth, 1152, 128, 2176, [17])]
        for gi, (ht, c0, n, s0, blks) in enumerate(groups):
            for d in range(2):
                for c in range(16):
                    P.op("pe", lambda E, d=d, c=c, ht=ht, c0=c0, n=n: E.matmul(
                        pl[d][d * 32:d * 32 + 16, 0:n], wlr_t[:, c, d * 16:(d + 1) * 16], ht[:, c, c0:c0 + n],
                        start=(c == 0), stop=(c == 15)),
                        reads=["wlr_t"] + [("hT", b) for b in blks], writes=[("pl", d)])
                P.op("dve", lambda E, d=d, n=n, s0=s0: E.tensor_copy(
                    lr[d * 32:d * 32 + 16, s0:s0 + n], pl[d][d * 32:d * 32 + 16, 0:n]),
                    reads=[("pl", d), "lr"], writes=[("lrw", d, gi)])
    lr_keys = [[("lrw", d, gi) for gi in range(6)] for d in range(2)]

    SCALE_Q = 1.0 / 16.0
    with Phase() as ph:
        k_h = sb(ph, "k_h", [128, NB, 256], BF16)
        v_h = sb(ph, "v_h", [128, NB, 512], BF16)
        q_h = sb(ph, "q_h", [128, NOWN, 256], BF16)
        o_acc = sb(ph, "o_acc", [128, NOWN, 512], F32)
        lf = [sb(ph, f"lf{i}", [128, 256]) for i in range(2)]
        ex1 = [sb(ph, f"ex1_{i}", [128, 256]) for i in range(2)]
        Einv = [sb(ph, f"Einv{i}", [128, 256]) for i in range(2)]
        Eq = [sb(ph, f"Eq{i}", [128, 256]) for i in range(2)]
        Est = [sb(ph, f"Est{i}", [128, 256]) for i in range(2)]
        k_in = [sb(ph, f"k_in{i}", [128, 256], BF16) for i in range(2)]
        q_in = [sb(ph, f"q_in{i}", [128, 256], BF16) for i in range(2)]
        k_st = [sb(ph, f"k_st{i}", [128, 256], BF16) for i in range(2)]
        kqT = [sb(ph, f"kqT{i}", [128, 4, 128], BF16) for i in range(2)]
        decay = [sb(ph, f"decay{i}", [128, 2, 2]) for i in range(2)]
        at_sb = [sb(ph, f"at_sb{i}", [128, 64], BF16) for i in range(2)]
        Sst = sb(ph, "Sst", [128, 2, 512], F32)
        Sbf = sb(ph, "Sbf", [128, 2, 512], BF16)
        o_fin = sb(ph, "o_fin", [128, 512], F32)
        junkg = sb(ph, "junkg", [128, 512], BF16)
        sr = sb(ph, "sr", [128, 512], F32)
        ogb = sb(ph, "ogb", [128, 512], BF16)
        ogT_st = [sb(ph, f"ogT_st{i}", [128, 4, 128], BF16) for i in range(2)]
        gss = sb(ph, "gss", [128, 2])
        grt = sb(ph, "grt", [128, 2])
        grs = sb(ph, "grs", [128, 2])
        pj = [BK[0], BK[1]]
        ptr = bf(2)
        pdec = BK[3]
        po = [BK[4], BK[5]]
        pds = [BK[6], BK[7]]
        pjc = [0]

        def pj_next():
            i = pjc[0] % 2
            pjc[0] += 1
            return pj[i], ("pj", i)

        for hd in range(4):
            wq, wqk_ = load_w(w_in[:, OFF_GQ + hd * 256:OFF_GQ + (hd + 1) * 256])
            wk, wkk_ = load_w(w_in[:, OFF_GK + hd * 256:OFF_GK + (hd + 1) * 256])
            wv0, wv0k = load_w(w_in[:, OFF_GV + hd * 512:OFF_GV + hd * 512 + 256])
            wv1, wv1k = load_w(w_in[:, OFF_GV + hd * 512 + 256:OFF_GV + (hd + 1) * 512])
            for blk in range(NB):
                ht, c0 = hT_blk(blk)
                own = 1 <= blk <= NOWN
                pt, pk = pj_next()
                for c in range(16):
                    P.op("pe", lambda E, pt=pt, ht=ht, c0=c0, c=c, wk=wk: E.matmul(
                        pt[:, 0:256], ht[:, c, c0:c0 + 128], wk[:, c, :], start=(c == 0), stop=(c == 15)),
                        reads=[("hT", blk), wkk_], writes=[pk])
                if own:
                    for c in range(16):
                        P.op("pe", lambda E, pt=pt, ht=ht, c0=c0, c=c, wq=wq: E.matmul(
                            pt[:, 256:512], ht[:, c, c0:c0 + 128], wq[:, c, :], start=(c == 0), stop=(c == 15)),
                            reads=[("hT", blk), wqk_], writes=[pk])
                P.op("act", lambda E, pt=pt, blk=blk: E.activation(k_h[:, blk, :], pt[:, 0:256], AF.Copy),
                     reads=[pk], writes=[("k_h", blk)])
                if own:
                    P.op("act", lambda E, pt=pt, blk=blk: E.activation(
                        q_h[:, blk - 1, :], pt[:, 256:512], AF.Copy, scale=SCALE_Q),
                        reads=[pk], writes=[("q_h", blk)])
                pt, pk = pj_next()
                for half_, (wv, wvk) in enumerate(((wv0, wv0k), (wv1, wv1k))):
                    for c in range(16):
                        P.op("pe", lambda E, pt=pt, ht=ht, c0=c0, c=c, wv=wv, half_=half_: E.matmul(
                            pt[:, half_ * 256:(half_ + 1) * 256], ht[:, c, c0:c0 + 128], wv[:, c, :],
                            start=(c == 0), stop=(c == 15)),
                            reads=[("hT", blk), wvk], writes=[pk])
                P.op("dve", lambda E, pt=pt, blk=blk: E.tensor_copy(v_h[:, blk, :], pt[:]),
                     reads=[pk], writes=[("v_h", blk)])
            wr0, wr0k = load_w(w_in[:, OFF_GR + hd * 512:OFF_GR + hd * 512 + 256])
            wr1, wr1k = load_w(w_in[:, OFF_GR + hd * 512 + 256:OFF_GR + (hd + 1) * 512])

            for d in range(2):
                if d == 0:
                    border = [0] + list(range(1, NOWN + 1))
                else:
                    border = list(range(NB - 1, 0, -1))
                first_state = True
                for bi, blk in enumerate(border):
                    own = 1 <= blk <= NOWN
                    par = bi % 2
                    kk = ("gp", par)
                    if d == 0:
                        chunks = [1] if blk == 0 else [0, 1]
                    else:
                        chunks = [0] if blk == NB - 1 else [1, 0]
                    pt, pk = pj_next()
                    P.op("pe", lambda E, pt=pt, d=d, blk=blk, hd=hd: E.matmul(
                        pt[:, 0:256], lr[d * 32:d * 32 + 32, blk * 128:(blk + 1) * 128],
                        w2a[d * 32:d * 32 + 32, hd * 256:(hd + 1) * 256], start=True, stop=True),
                        reads=lr_keys[d] + ["w2a0", "w2a1"], writes=[pk])
                    P.op("act", lambda E, pt=pt, par=par: E.activation(ex1[par][:], pt[:, 0:256], AF.Exp, scale=-1.0),
                         reads=[pk], writes=[("ex1", par)])
                    P.op("act", lambda E, par=par: E.activation(lf[par][:], ex1[par][:], AF.Ln, bias=1.0),
                         reads=[("ex1", par)], writes=[("lf", par)])
                    P.op("pe", lambda E, pt=pt, d=d, par=par: E.matmul(
                        pt[:, 256:512], tri_t[:, 2 + d, :], lf[par][:], start=True, stop=True),
                        reads=[("lf", par), "tri_t"], writes=[pk])
                    P.op("act", lambda E, pt=pt, par=par: E.activation(
                        Est[par][:], pt[:, 256:512], AF.Exp, scale=-1.0 / 16.0),
                        reads=[pk], writes=[("Est", par)])
                    P.op("dve", lambda E, par=par, blk=blk: E.tensor_tensor(
                        k_st[par][:], k_h[:, blk, :], Est[par][:], ALU.mult),
                        reads=[("k_h", blk), ("Est", par)], writes=[("k_st", par)])
                    for dc in range(2):
                        P.op("pe", lambda E, dc=dc, par=par: E.matmul(
                            pdec[:, dc * 2:dc * 2 + 2], lf[par][:, dc * 128:(dc + 1) * 128], ind2_t[:],
                            start=True, stop=True),
                            reads=[("lf", par), "ind2_t"], writes=["pdec_d"])
                    P.op("act", lambda E, par=par: E.activation(
                        decay[par][:].rearrange("p a b -> p (a b)"), pdec[:, 0:4], AF.Exp, scale=-1.0 / 16.0),
                        reads=["pdec_d"], writes=[("decay", par)])
                    if own:
                        pt2, pk2 = pj_next()
                        P.op("pe", lambda E, pt2=pt2, d=d, par=par: E.matmul(
                            pt2[:, 0:256], tri_t[:, d, :], lf[par][:], start=True, stop=True),
                            reads=[("lf", par), "tri_t"], writes=[pk2])
                        P.op("act", lambda E, pt2=pt2, par=par: E.activation(
                            Einv[par][:], pt2[:, 0:256], AF.Exp, scale=1.0 / 16.0),
                            reads=[pk2], writes=[("Einv", par)])
                        P.op("act", lambda E, pt2=pt2, par=par: E.activation(
                            Eq[par][:], pt2[:, 0:256], AF.Exp, scale=-1.0 / 16.0),
                            reads=[pk2], writes=[("Eq", par)])
                        P.op("dve", lambda E, par=par, blk=blk: E.tensor_tensor(
                            k_in[par][:], k_h[:, blk, :], Einv[par][:], ALU.mult),
                            reads=[("k_h", blk), ("Einv", par)], writes=[("k_in", par)])
                        P.op("dve", lambda E, par=par, blk=blk: E.tensor_tensor(
                            q_in[par][:], q_h[:, blk - 1, :], Eq[par][:], ALU.mult),
                            reads=[("q_h", blk), ("Eq", par)], writes=[("q_in", par)])
                        for j in range(2):
                            P.op("pe", lambda E, j=j, par=par: E.transpose(
                                ptr[:, j, :], k_in[par][:, j * 128:(j + 1) * 128], ident_bf[:]),
                                reads=[("k_in", par), "ident_bf"], writes=["ptr"])
                        for j in range(2):
                            P.op("pe", lambda E, j=j, par=par: E.transpose(
                                ptr[:, 2 + j, :], q_in[par][:, j * 128:(j + 1) * 128], ident_bf[:]),
                                reads=[("q_in", par), "ident_bf"], writes=["ptr"])
                        P.op("dve", lambda E, par=par: E.tensor_copy(kqT[par][:], ptr[:, 0:4, :]),
                             reads=["ptr"], writes=[("kqT", par)])
                    pob, pobk = po[par], ("po", par)
                    for cc in chunks:
                        sl = slice(cc * 64, cc * 64 + 64)
                        if own:
                            for dc in range(2):
                                P.op("pe", lambda E, dc=dc, sl=sl, par=par: E.matmul(
                                    pdec[sl, 64:128], kqT[par][:, dc, sl], kqT[par][:, 2 + dc, sl],
                                    start=(dc == 0), stop=(dc == 1)),
                                    reads=[("kqT", par)], writes=[("pdec_a", cc)])
                            P.op("dve", lambda E, sl=sl, par=par, d=d: E.tensor_tensor(
                                at_sb[par][sl, :], pdec[sl, 64:128], mask_t[sl, d, :], ALU.mult),
                                reads=[("pdec_a", cc), "mask_t"], writes=[("at_sb", par, cc)])
                            P.op("pe", lambda E, sl=sl, par=par, blk=blk, pob=pob, fs=first_state: E.matmul(
                                pob[sl, :], at_sb[par][sl, :], v_h[sl, blk, :], start=True, stop=fs),
                                reads=[("at_sb", par, cc), ("v_h", blk)], writes=[(pobk, cc)])
                            if not first_state:
                                for dc in range(2):
                                    P.op("pe", lambda E, sl=sl, par=par, dc=dc, pob=pob: E.matmul(
                                        pob[sl, :], kqT[par][:, 2 + dc, sl], Sbf[:, dc, :],
                                        start=False, stop=(dc == 1)),
                                        reads=[("kqT", par), "Sbf"], writes=[(pobk, cc)])
                        for dc in range(2):
                            P.op("pe", lambda E, sl=sl, par=par, dc=dc, blk=blk: E.matmul(
                                pds[dc][:], k_st[par][sl, dc * 128:(dc + 1) * 128], v_h[sl, blk, :],
                                start=True, stop=True),
                                reads=[("k_st", par), ("v_h", blk)], writes=[("pds", dc)])
                            if first_state:
                                P.op("dve", lambda E, dc=dc: E.tensor_copy(Sst[:, dc, :], pds[dc][:]),
                                     reads=[("pds", dc)], writes=[("Sst", dc)])
                            else:
                                P.op("dve", lambda E, dc=dc, par=par, cc=cc: E.scalar_tensor_tensor(
                                    Sst[:, dc, :], Sst[:, dc, :], decay[par][:, dc, cc:cc + 1], pds[dc][:],
                                    ALU.mult, ALU.add),
                                    reads=[("pds", dc), ("Sst", dc), ("decay", par)], writes=[("Sst", dc)])
                        P.op("act", lambda E: E.activation(Sbf[:], Sst[:], AF.Copy),
                             reads=[("Sst", 0), ("Sst", 1)], writes=["Sbf"])
                        first_state = False
                    if own:
                        ob = blk - 1
                        okeys = [(pobk, 0), (pobk, 1)]
                        if d == 0:
                            P.op("act", lambda E, ob=ob, pob=pob: E.activation(o_acc[:, ob, :], pob[:], AF.Copy),
                                 reads=okeys, writes=[("o_acc", ob)])
                        else:
                            P.op("dve", lambda E, ob=ob, pob=pob: E.tensor_tensor(
                                o_fin[:], pob[:], o_acc[:, ob, :], ALU.add),
                                reads=okeys + [("o_acc", ob)], writes=["o_fin"])
                            P.op("act", lambda E, par=par: E.activation(
                                junkg[:], o_fin[:], AF.Square, accum_out=gss[:, par:par + 1]),
                                reads=["o_fin"], writes=["junkg", ("gss", par)])
                            P.op("act", lambda E, par=par: E.activation(
                                grt[:, par:par + 1], gss[:, par:par + 1], AF.Sqrt, scale=1.0 / 512.0, bias=EPS),
                                reads=[("gss", par)], writes=[("grt", par)])
                            P.op("dve", lambda E, par=par: E.reciprocal(grs[:, par:par + 1], grt[:, par:par + 1]),
                                 reads=[("grt", par)], writes=[("grs", par)])
                            ht, c0 = hT_blk(blk)
                            pt, pk = pj_next()
                            for half_, (wr, wrk) in enumerate(((wr0, wr0k), (wr1, wr1k))):
                                for c in range(16):
                                    P.op("pe", lambda E, pt=pt, ht=ht, c0=c0, c=c, wr=wr, half_=half_: E.matmul(
                                        pt[:, half_ * 256:(half_ + 1) * 256], ht[:, c, c0:c0 + 128], wr[:, c, :],
                                        start=(c == 0), stop=(c == 15)),
                                        reads=[("hT", blk), wrk], writes=[pk])
                            P.op("act", lambda E, pt=pt: E.activation(sr[:], pt[:], AF.Silu),
                                 reads=[pk], writes=["sr"])
                            P.op("dve", lambda E, par=par: E.scalar_tensor_tensor(
                                o_fin[:], o_fin[:], grs[:, par:par + 1], glagn[:], ALU.mult, ALU.mult),
                                reads=["o_fin", ("grs", par), "glagn"], writes=["o_fin"])
                            P.op("dve", lambda E: E.tensor_tensor(ogb[:], o_fin[:], sr[:], ALU.mult),
                                 reads=["o_fin", "sr"], writes=["ogb"])
                            for j in range(4):
                                P.op("pe", lambda E, j=j: E.transpose(
                                    ptr[:, 4 + j, :], ogb[:, j * 128:(j + 1) * 128], ident_bf[:]),
                                    reads=["ogb", "ident_bf"], writes=["ptr2"])
                            P.op("act", lambda E, par=par: E.activation(ogT_st[par][:], ptr[:, 4:8, :], AF.Copy),
                                 reads=["ptr2"], writes=[("ogT_st", par)])
                            P.dma("sp", ogT_d[hd * 512:(hd + 1) * 512, ob * 128:(ob + 1) * 128].rearrange(
                                "(j p) t -> p j t", p=128), ogT_st[par][:],
                                reads=[("ogT_st", par)], writes=[("ogT_d", hd, ob)])
    if stop_after == "gla":
        return finish_dbg(nc, P, out, [tabs, oth, att, top])

    SC = 1.0 / math.sqrt(128.0)
    with Phase() as ph:
        kT_h = sb(ph, "kT_h", [128, 2, S], BF16)
        qT_h = sb(ph, "qT_h", [128, 2, TOK], BF16)
        V_h = sb(ph, "V_h", [128, NB, 258], BF16)
        if stop_after == "dstart0":
            return finish_dbg(nc, P, out, [])
        import os
        if "noop" in os.environ.get("DBG_DIFF", ""):
            pass
        elif "novalid2" in os.environ.get("DBG_DIFF", ""):
            P.op("pool", lambda E: E.memset(V_h[:], 1.0), writes=["V_valid0", "V_valid1"])
        elif "novalid3" in os.environ.get("DBG_DIFF", ""):
            P.op("dve", lambda E: E.memset(kT_h[:, 0, 0:64], 1.0), writes=["V_valid0", "V_valid1"])
        elif "novalid" in os.environ.get("DBG_DIFF", ""):
            P.op("dve", lambda E: E.memset(V_h[:], 1.0), writes=["V_valid0", "V_valid1"])
        else:
            P.op("dve", lambda E: E.tensor_copy(V_h[:, :, 256], valid_t[:]), reads=["valid_t"], writes=["V_valid0"])
            P.op("dve", lambda E: E.tensor_copy(V_h[:, :, 257], valid_t[:]), reads=["valid_t"], writes=["V_valid1"])
        ktm = [sb(ph, f"ktm{i}", [128, 2, 128], BF16) for i in range(2)]
        qtm = [sb(ph, f"qtm{i}", [128, 2, 128], BF16) for i in range(2)]
        rtmp = [sb(ph, f"rtmp{i}", [128, 2, 4, 16]) for i in range(2)]
        rstage = [sb(ph, f"rstage{i}", [128, 256]) for i in range(2)]
        cs2_t = sb(ph, "cs2_t", [128, NB, 64])
        P.op("dve", lambda E: E.tensor_copy(cs2_t[:, :, 0:16], cos_t[:]), reads=["cos_t"], writes=[("cs2", 0)])
        P.op("dve", lambda E: E.tensor_copy(cs2_t[:, :, 16:32], cos_t[:]), reads=["cos_t"], writes=[("cs2", 1)])
        P.op("dve", lambda E: E.tensor_scalar(cs2_t[:, :, 32:48], sin_t[:], -1.0, None, ALU.mult), reads=["sin_t"], writes=[("cs2", 2)])
        P.op("dve", lambda E: E.tensor_copy(cs2_t[:, :, 48:64], sin_t[:]), reads=["sin_t"], writes=["cs2_t"])
        PT = [sb(ph, f"PT{i}", [128, 2, 256], BF16) for i in range(3)]
        rz = sb(ph, "rz", [128, 4])
        t1 = sb(ph, "t1", [128, 256])
        od = sb(ph, "od", [128, 256])
        junkd = sb(ph, "junkd", [128, 256], BF16)
        dss = sb(ph, "dss", [128, 2])
        drt = sb(ph, "drt", [128, 2])
        drs = sb(ph, "drs", [128, 2])
        odb = sb(ph, "odb", [128, 256], BF16)
        odT_st = [sb(ph, f"odT_st{i}", [128, 2, 128], BF16) for i in range(2)]
        if stop_after == "dstart1":
            return finish_dbg(nc, P, out, [])
        pst2_ = [BK[0], BK[1]]
        pst_ = [t[:].rearrange("p (m q) -> p m q", m=2) for t in pst2_]
        ptr = bf(2)
        pO = [BK[3], BK[4], BK[5], BK[6]]
        pjc = [0]

        def pj_next():
            i = pjc[0] % 2
            pjc[0] += 1
            return pst2_[i][:], ("ast", i)

        import os
        FLAGS = os.environ.get("DBG_DIFF", "")

        def rope(dst, src_ps, blk, par, key_in, key_out):
            if "norope" in FLAGS:
                P.op("act", lambda E: E.activation(dst[:].rearrange("p m d -> p (m d)"), src_ps, AF.Copy),
                     reads=[key_in], writes=[(key_out, "c")])
                return [(key_out, "c")]
            return rope_(dst, src_ps, blk, par, key_in, key_out)

        def rope_(dst, src_ps, blk, par, key_in, key_out):
            r = rtmp[par]
            st = rstage[par]
            rk = ("rtmp", par)
            sk = ("rstage", par)
            P.op("act", lambda E: E.activation(st[:], src_ps, AF.Copy), reads=[key_in], writes=[sk])
            P.op("act", lambda E: E.activation(dst[:].rearrange("p m d -> p (m d)"), st[:], AF.Copy),
                 reads=[sk], writes=[(key_out, "c")])
            keys = [(key_out, "c")]
            c2 = cs2_t[:, blk, 0:32]
            s2 = cs2_t[:, blk, 32:64]
            for m in range(2):
                rr = r[:, m].rearrange("p a b -> p (a b)")
                x = st[:, m * 128:m * 128 + 32]
                P.op("dve", lambda E, rr=rr, m=m: E.tensor_copy(rr[:, 0:16], st[:, m * 128 + 16:m * 128 + 32]),
                     reads=[sk], writes=[(rk, m, 0)])
                P.op("dve", lambda E, rr=rr, m=m: E.tensor_copy(rr[:, 16:32], st[:, m * 128:m * 128 + 16]),
                     reads=[sk], writes=[(rk, m, 1)])
                P.op("dve", lambda E, rr=rr: E.tensor_tensor(rr[:, 0:32], rr[:, 0:32], s2, ALU.mult),
                     reads=[(rk, m, 0), (rk, m, 1), "cs2_t", ("cs2", 0), ("cs2", 1), ("cs2", 2)], writes=[(rk, m, 2)])
                P.op("dve", lambda E, rr=rr, x=x: E.tensor_tensor(rr[:, 32:64], x, c2, ALU.mult),
                     reads=[sk, "cs2_t", ("cs2", 0), ("cs2", 1), ("cs2", 2)], writes=[(rk, m, 3)])
                P.op("dve", lambda E, rr=rr, m=m: E.tensor_tensor(dst[:, m, 0:32], rr[:, 0:32], rr[:, 32:64], ALU.add),
                     reads=[(rk, m, 2), (rk, m, 3), (key_out, "c")], writes=[(key_out, "a", m)])
                keys += [(key_out, "a", m)]
            return keys

        if stop_after == "dstart":
            return finish_dbg(nc, P, out, [])
        for h in range(8):
            wq, wqk_ = load_w(w_in[:, OFF_DQ + h * 256:OFF_DQ + (h + 1) * 256])
            wk, wkk_ = load_w(w_in[:, OFF_DK + h * 256:OFF_DK + (h + 1) * 256])
            wv, wvk_ = load_w(w_in[:, OFF_DV + h * 256:OFF_DV + (h + 1) * 256])
            for blk in range(NB):
                ht, c0 = hT_blk(blk)
                own = 1 <= blk <= NOWN
                par = blk % 2
                pt, pk = pj_next()
                for c in range(16):
                    P.op("pe", lambda E, pt=pt, ht=ht, c0=c0, c=c, wk=wk: E.matmul(
                        pt[:, 0:256], ht[:, c, c0:c0 + 128], wk[:, c, :], start=(c == 0), stop=(c == 15)),
                        reads=[("hT", blk), wkk_], writes=[pk])
                for c in range(16):
                    P.op("pe", lambda E, pt=pt, ht=ht, c0=c0, c=c, wv=wv: E.matmul(
                        pt[:, 256:512], ht[:, c, c0:c0 + 128], wv[:, c, :], start=(c == 0), stop=(c == 15)),
                        reads=[("hT", blk), wvk_], writes=[pk])
                P.op("act", lambda E, pt=pt, blk=blk: E.activation(V_h[:, blk, 0:256], pt[:, 256:512], AF.Copy),
                     reads=[pk], writes=[("V_h", blk)])
                kkeys = rope(ktm[par], pt[:, 0:256], blk, par, pk, ("ktm", par))
                for m in range(2):
                    P.op("pe", lambda E, m=m, par=par: E.transpose(ptr[:, m, :], ktm[par][:, m, :], ident_bf[:]),
                         reads=kkeys + ["ident_bf"], writes=["aptr_k"])
                P.op("act", lambda E, blk=blk: E.activation(
                    kT_h[:, :, blk * 128:(blk + 1) * 128], ptr[:, 0:2, :], AF.Copy),
                    reads=["aptr_k"], writes=[("kT_h", blk)])
                if own:
                    pt, pk = pj_next()
                    for c in range(16):
                        P.op("pe", lambda E, pt=pt, ht=ht, c0=c0, c=c, wq=wq: E.matmul(
                            pt[:, 0:256], ht[:, c, c0:c0 + 128], wq[:, c, :], start=(c == 0), stop=(c == 15)),
                            reads=[("hT", blk), wqk_], writes=[pk])
                    qkeys = rope(qtm[par], pt[:, 0:256], blk, par, pk, ("qtm", par))
                    for m in range(2):
                        P.op("pe", lambda E, m=m, par=par: E.transpose(ptr[:, 2 + m, :], qtm[par][:, m, :], ident_bf[:]),
                             reads=qkeys + ["ident_bf"], writes=["aptr_q"])
                    P.op("dve", lambda E, blk=blk: E.tensor_copy(
                        qT_h[:, :, (blk - 1) * 128:blk * 128], ptr[:, 2:4, :]),
                        reads=["aptr_q"], writes=[("qT_h", blk - 1)])
            if stop_after == "dproj":
                d1 = dout("dbg_kT", [128, 2, S], BF16)
                d2 = dout("dbg_qT", [128, 2, TOK], BF16)
                d3 = dout("dbg_V", [128, NB, 258], BF16)
                P.dma("sp", d1, kT_h[:], reads=[("kT_h", b) for b in range(NB)], is_output=True)
                P.dma("sp", d2, qT_h[:], reads=[("qT_h", b) for b in range(NOWN)], is_output=True)
                P.dma("sp", d3, V_h[:], reads=[("V_h", b) for b in range(NB)] + ["V_valid0", "V_valid1"], is_output=True)
                return finish_dbg(nc, P, out, [])
            pti = 0
            for qg in range(4):
                for kb in range(NB):
                    stp = kb % 2
                    for m in range(2):
                        P.op("pe", lambda E, m=m, stp=stp, kb=kb, qg=qg: E.matmul(
                            pst_[stp][:, m, :], kT_h[:, m, kb * 128:(kb + 1) * 128],
                            qT_h[:, m, qg * 256:(qg + 1) * 256], start=True, stop=True),
                            reads=[("kT_h", kb), ("qT_h", 2 * qg), ("qT_h", 2 * qg + 1)], writes=[("ast", stp)])
                    pp = pti % 3
                    pti += 1
                    P.op("act", lambda E, pp=pp, stp=stp: E.activation(PT[pp][:], pst_[stp], AF.Exp, scale=SC),
                         reads=[("ast", stp)], writes=[("PT", pp)])
                    for j in range(2):
                        for m in range(2):
                            P.op("pe", lambda E, j=j, m=m, pp=pp, kb=kb: E.matmul(
                                pO[j * 2 + m][:, 0:258], PT[pp][:, m, j * 128:(j + 1) * 128], V_h[:, kb, :],
                                start=(kb == 0), stop=(kb == NB - 1)),
                                reads=[("PT", pp), ("V_h", kb), "V_valid0", "V_valid1"], writes=[("aO", j, m)])
                for j in range(2):
                    qb = qg * 2 + j
                    par = j
                    P.op("dve", lambda E, j=j: E.reciprocal(rz[:, j * 2:j * 2 + 1], pO[j * 2][:, 256:257]),
                         reads=[("aO", j, 0)], writes=[("rz", j, 0)])
                    P.op("dve", lambda E, j=j: E.reciprocal(rz[:, j * 2 + 1:j * 2 + 2], pO[j * 2 + 1][:, 256:257]),
                         reads=[("aO", j, 1)], writes=[("rz", j, 1)])
                    P.op("dve", lambda E, j=j: E.tensor_scalar(
                        t1[:], pO[j * 2 + 1][:, 0:256], rz[:, j * 2 + 1:j * 2 + 2], neglam[:, 0:1], ALU.mult, ALU.mult),
                        reads=[("aO", j, 1), ("rz", j, 1), "neglam"], writes=["t1"])
                    P.op("dve", lambda E, j=j: E.scalar_tensor_tensor(
                        od[:], pO[j * 2][:, 0:256], rz[:, j * 2:j * 2 + 1], t1[:], ALU.mult, ALU.add),
                        reads=[("aO", j, 0), ("rz", j, 0), "t1"], writes=["od"])
                    P.op("act", lambda E, par=par: E.activation(
                        junkd[:], od[:], AF.Square, accum_out=dss[:, par:par + 1]),
                        reads=["od"], writes=["junkd", ("dss", par)])
                    P.op("act", lambda E, par=par: E.activation(
                        drt[:, par:par + 1], dss[:, par:par + 1], AF.Sqrt, scale=1.0 / 256.0, bias=EPS),
                        reads=[("dss", par)], writes=[("drt", par)])
                    P.op("dve", lambda E, par=par: E.reciprocal(drs[:, par:par + 1], drt[:, par:par + 1]),
                         reads=[("drt", par)], writes=[("drs", par)])
                    P.op("dve", lambda E, par=par: E.scalar_tensor_tensor(
                        odb[:], od[:], drs[:, par:par + 1], diffgn[:], ALU.mult, ALU.mult),
                        reads=["od", ("drs", par), "diffgn"], writes=["odb"])
                    for i2 in range(2):
                        P.op("pe", lambda E, i2=i2: E.transpose(
                            ptr[:, 4 + i2, :], odb[:, i2 * 128:(i2 + 1) * 128], ident_bf[:]),
                            reads=["odb", "ident_bf"], writes=["aptr_o"])
                    P.op("act", lambda E, par=par: E.activation(odT_st[par][:], ptr[:, 4:6, :], AF.Copy),
                         reads=["aptr_o"], writes=[("odT_st", par)])
                    P.dma("sp", odT_d[h * 256:(h + 1) * 256, qb * 128:(qb + 1) * 128].rearrange(
                        "(j p) t -> p j t", p=128), odT_st[par][:],
                        reads=[("odT_st", par)], writes=[("odT_d", h, qb)])
    if stop_after == "attn":
        return finish_dbg(nc, P, out, [tabs, oth, att, top])
    P.barrier()
    tabs.close()
    oth.close()

    with Phase() as ph:
        yT = sb(ph, "yT", [128, 16, TOK], BF16)
        with Phase() as ph2:
            ring2 = Ring([sb(ph2, f"wringD{i}", [128, 16, 256], BF16) for i in range(4)], "wringD")
            og_g = sb(ph2, "og_g", [128, 16, TOK], BF16)
            od_g = sb(ph2, "od_g", [128, 16, TOK], BF16)
            P.dma("sp", og_g[:], ogT_d.rearrange("(c p) t -> p c t", p=128),
                  reads=[("ogT_d", hd, ob) for hd in range(4) for ob in range(8)], writes=["og_g"])
            P.dma("sp", od_g[:], odT_d.rearrange("(c p) t -> p c t", p=128),
                  reads=[("odT_d", h, qb) for h in range(8) for qb in range(8)], writes=["od_g"])
            sa = [sb(ph2, f"sa{i}", [128, 512]) for i in range(2)]
            sbb = [sb(ph2, f"sbb{i}", [128, 512]) for i in range(2)]
            ta = [sb(ph2, f"ta{i}", [128, 512]) for i in range(2)]
            tb_ = [sb(ph2, f"tb{i}", [128, 512]) for i in range(2)]
            pU = BK
            own_keys = [("hT", b) for b in range(1, 9)]
            it = 0
            for f2 in range(8):
                wa_t, wa_k = load_w(w_a[:, f2 * 256:(f2 + 1) * 256], ring_=ring2)
                wb_t, wb_k = load_w(w_b[:, f2 * 256:(f2 + 1) * 256], ring_=ring2)
                wza_t, wza_k = load_w(w_in[:, OFF_ZA + f2 * 256:OFF_ZA + (f2 + 1) * 256], ring_=ring2)
                wzb_t, wzb_k = load_w(w_in[:, OFF_ZB + f2 * 256:OFF_ZB + (f2 + 1) * 256], ring_=ring2)
                for j in range(2):
                    fc = f2 * 2 + j
                    for tg in range(2):
                        par = it % 2
                        it += 1
                        banks = pU[par * 4:(par + 1) * 4]
                        bk = [("dU", par, q) for q in range(4)]
                        srcs = [(wa_t, wa_k, og_g, ["og_g"]), (wb_t, wb_k, od_g, ["od_g"]),
                                (wza_t, wza_k, hT_own, own_keys), (wzb_t, wzb_k, hT_own, own_keys)]
                        for q, (wt, wk_, act_t, akeys) in enumerate(srcs):
                            for c in range(16):
                                P.op("pe", lambda E, q=q, wt=wt, act_t=act_t, c=c, j=j, tg=tg, banks=banks: E.matmul(
                                    banks[q][:], wt[:, c, j * 128:(j + 1) * 128], act_t[:, c, tg * 512:(tg + 1) * 512],
                                    start=(c == 0), stop=(c == 15)),
                                    reads=[wk_] + akeys, writes=[bk[q]])
                        P.op("act", lambda E, par=par, banks=banks: E.activation(sa[par][:], banks[2][:], AF.Sigmoid),
                             reads=[bk[2]], writes=[("sa", par)])
                        P.op("act", lambda E, par=par, banks=banks: E.activation(sbb[par][:], banks[3][:], AF.Sigmoid),
                             reads=[bk[3]], writes=[("sbb", par)])
                        P.op("dve", lambda E, par=par, banks=banks: E.tensor_tensor(ta[par][:], banks[0][:], sa[par][:], ALU.mult),
                             reads=[bk[0], ("sa", par)], writes=[("ta", par)])
                        P.op("dve", lambda E, par=par, banks=banks: E.tensor_tensor(tb_[par][:], banks[1][:], sbb[par][:], ALU.mult),
                             reads=[bk[1], ("sbb", par)], writes=[("tb", par)])
                        P.op("dve", lambda E, par=par, fc=fc, tg=tg: E.tensor_tensor(
                            yT[:, fc, tg * 512:(tg + 1) * 512], ta[par][:], tb_[par][:], ALU.add),
                            reads=[("ta", par), ("tb", par)], writes=[("yT", fc, tg)])
        with Phase() as ph2:
            ring3 = Ring([sb(ph2, f"wringO{i}", [128, 16, 256], BF16) for i in range(3)], "wringO")
            xres = [sb(ph2, f"xres{i}", [128, 256]) for i in range(3)]
            hsb = [sb(ph2, f"hsb{i}", [128, 256]) for i in range(3)]
            pH = [BK[0], BK[1]]
            ykeys = [("yT", fc, tg) for fc in range(16) for tg in range(2)]
            it = 0
            for nt in range(8):
                wo_t, wo_k = load_w(w_o[:, nt * 256:(nt + 1) * 256], ring_=ring3)
                for ob in range(NOWN):
                    par = it % 2
                    p3 = it % 3
                    it += 1
                    P.dma("sp", xres[p3][:], xs[(ob + 1) * 128:(ob + 2) * 128, nt * 256:(nt + 1) * 256],
                          writes=[("xres", p3)])
                    for c in range(16):
                        P.op("pe", lambda E, par=par, c=c, ob=ob, wo_t=wo_t: E.matmul(
                            pH[par][:, 0:256], yT[:, c, ob * 128:(ob + 1) * 128], wo_t[:, c, :],
                            start=(c == 0), stop=(c == 15)),
                            reads=[wo_k] + ykeys, writes=[("dH", par)])
                    P.op("dve", lambda E, par=par, p3=p3: E.tensor_tensor(
                        hsb[p3][:], pH[par][:, 0:256], xres[p3][:], ALU.add),
                        reads=[("dH", par), ("xres", p3)], writes=[("hsb", p3)])
                    P.dma("sp", hs1_d[ob * 128:(ob + 1) * 128, nt * 256:(nt + 1) * 256], hsb[p3][:],
                          reads=[("hsb", p3)], writes=[("hs1_d", ob, nt)])
    att.close()
    if stop_after == "hs1":
        return finish_dbg(nc, P, out, [top])

    hs1_keys = [("hs1_d", ob, nt) for ob in range(8) for nt in range(8)]

    peer = ExitStack()
    xnT = sb(peer, "xnT", [128, 16, TOK], BF16)
    with Phase() as ph:
        gm1 = sb(ph, "gf_bc", [128, D], F32)
        P.dma("sp", gm1[:], gvec[1:2, :].partition_broadcast(128), writes=["gf"])
        xt1 = [sb(ph, f"pxt{i}", [128, D], F32) for i in range(2)]
        hb1 = [sb(ph, f"phb{i}", [128, D], BF16) for i in range(2)]
        junk1 = sb(ph, "pjunk", [128, D], BF16)
        tmp1 = (sb(ph, "pss", [128, 2]), sb(ph, "prt", [128, 2]), sb(ph, "prstd", [128, 2]))
        tp1 = [bf(0), bf(1)]
        for ob in range(NOWN):
            i = ob % 2
            P.dma("sp", xt1[i][:], hs1_d[ob * 128:(ob + 1) * 128, :],
                  reads=[("hs1_d", ob, nt) for nt in range(8)], writes=[("pxt", i)])
            norm_rows(xt1[i][:], ("pxt", i), gm1, "gf", hb1[i][:], ("phb", i), junk1, tmp1, i)
            for h8 in range(2):
                for j in range(8):
                    c = h8 * 8 + j
                    P.op("pe", lambda E, h8=h8, j=j, c=c, i=i: E.transpose(
                        tp1[h8][:, j, :], hb1[i][:, c * 128:(c + 1) * 128], ident_bf[:]),
                        reads=[("phb", i), "ident_bf"], writes=[("ptp", h8)])
                if h8 == 0:
                    P.op("dve", lambda E, h8=h8, ob=ob: E.tensor_copy(
                        xnT[:, h8 * 8:(h8 + 1) * 8, ob * 128:(ob + 1) * 128], tp1[h8][:]),
                        reads=[("ptp", h8)], writes=[("xnT", ob)])
                else:
                    P.op("act", lambda E, h8=h8, ob=ob: E.activation(
                        xnT[:, h8 * 8:(h8 + 1) * 8, ob * 128:(ob + 1) * 128], tp1[h8][:], AF.Copy),
                        reads=[("ptp", h8)], writes=[("xnT", ob)])
    xn_keys = [("xnT", ob) for ob in range(NOWN)]

    with Phase() as ph:
        qT = sb(ph, "pqT", [128, 16, TOK], BF16)
        skT = sb(ph, "skT", [128, 16, 128], BF16)
        with Phase() as ph2:
            ring4 = Ring([sb(ph2, f"wringQ{i}", [128, 16, 256], BF16) for i in range(3)], "wringQ")
            skf = sb(ph2, "skf", [128, 16, 128], F32)
            P.dma("sp", skf[:], subk.rearrange("a n c -> n a c"), writes=["skf"])
            pq = [BK[0], BK[1]]
            ptk = f3(2, 4)
            for a4 in range(4):
                for j in range(4):
                    P.op("pe", lambda E, a4=a4, j=j: E.transpose(ptk[:, j, :], skf[:, a4 * 4 + j, :], ident_f[:]),
                         reads=["skf", "ident_f"], writes=["ptk"])
                P.op("dve", lambda E, a4=a4: E.tensor_copy(skT[:, a4 * 4:(a4 + 1) * 4, :], ptk[:]),
                     reads=["ptk"], writes=[("skT", a4)])
            it = 0
            for w2_ in range(8):
                wq_t, wq_k = load_w(w_q[:, w2_ * 256:(w2_ + 1) * 256], ring_=ring4)
                for j in range(2):
                    hp = w2_ * 2 + j
                    for tg in range(2):
                        par = it % 2
                        it += 1
                        for c in range(16):
                            P.op("pe", lambda E, par=par, c=c, j=j, tg=tg, wq_t=wq_t: E.matmul(
                                pq[par][:], wq_t[:, c, j * 128:(j + 1) * 128], xnT[:, c, tg * 512:(tg + 1) * 512],
                                start=(c == 0), stop=(c == 15)),
                                reads=[wq_k] + xn_keys, writes=[("pq", par)])
                        if tg == 0:
                            P.op("act", lambda E, par=par, hp=hp, tg=tg: E.activation(
                                qT[:, hp, tg * 512:(tg + 1) * 512], pq[par][:], AF.Copy),
                                reads=[("pq", par)], writes=[("pqT", hp, tg)])
                        else:
                            P.op("dve", lambda E, par=par, hp=hp, tg=tg: E.tensor_copy(
                                qT[:, hp, tg * 512:(tg + 1) * 512], pq[par][:]),
                                reads=[("pq", par)], writes=[("pqT", hp, tg)])
        skT_keys = [("skT", a4) for a4 in range(4)]
        with Phase() as ph2:
            s_sb = sb(ph2, "s_sb", [128, 16, 128])
            s_w = sb(ph2, "s_w", [128, 16, 128])
            sv = sb(ph2, "sv", [128, 16, 16])
            si = sb(ph2, "si", [128, 16, 16], U32)
            sif = sb(ph2, "sif", [128, 16, 16])
            cand = sb(ph2, "cand", [128, 8, 256])
            cand_w = sb(ph2, "cand_w", [128, 8, 256])
            cv = sb(ph2, "cv", [128, 8, 16])
            ci = sb(ph2, "ci", [128, 8, 16], U32)
            k12u = sb(ph2, "k12u", [128, 2, 8, 16], U32)
            k12f = sb(ph2, "k12f", [128, 2, 8, 16])
            eq = sb(ph2, "eq", [128, 8, 16, 16])
            IJ = sb(ph2, "IJ", [128, 3, 128])
            cvm = sb(ph2, "cvm", [128, 8, 16])
            zs = sb(ph2, "zs", [128, 8])
            rzs = sb(ph2, "rzs", [128, 8])
            IJT = sb(ph2, "IJT", [128, 3, 128])
            NAB = 8
            A_t = [sb(ph2, f"A_t{i}", [128, 128], BF16) for i in range(NAB)]
            B_t = [sb(ph2, f"B_t{i}", [128, 128], BF16) for i in range(NAB)]
            WT = [sb(ph2, f"WT{i}", [128, 128, 128], BF16) for i in range(2)]
            ps_s = [f3(i, 4) for i in range(4)]
            ps_w = [f3(4, 4), f3(5, 4)]
            ps_t = f3(6, 4)[:, 0:3, :]
            iota16 = iota_f[:, 0:16]
            for tb in range(NOWN):
                wpar = tb % 2
                for hp in range(16):
                    P.op("pe", lambda E, hp=hp, tb=tb: E.matmul(
                        ps_s[hp // 4][:, hp % 4, :], qT[:, hp, tb * 128:(tb + 1) * 128], skT[:, hp, :],
                        start=True, stop=True),
                        reads=[("pqT", hp, tb // 4)] + skT_keys, writes=[("ps_s", hp // 4)])
                for b4 in range(4):
                    P.op("act", lambda E, b4=b4: E.activation(s_sb[:, b4 * 4:(b4 + 1) * 4, :], ps_s[b4][:], AF.Copy),
                         reads=[("ps_s", b4)], writes=[("s_sb", b4)])
                for hp in range(16):
                    sk_ = ("s_sb", hp // 4)
                    P.op("dve", lambda E, hp=hp: E.max(sv[:, hp, 0:8], s_sb[:, hp, :]),
                         reads=[sk_], writes=[("sv", hp, 0)])
                    P.op("dve", lambda E, hp=hp: E.max_index(si[:, hp, 0:8], sv[:, hp, 0:8], s_sb[:, hp, :]),
                         reads=[sk_, ("sv", hp, 0)], writes=[("si", hp, 0)])
                    P.op("dve", lambda E, hp=hp: E.match_replace(s_w[:, hp, :], sv[:, hp, 0:8], s_sb[:, hp, :], -1e30),
                         reads=[sk_, ("sv", hp, 0)], writes=[("s_w", hp)])
                    P.op("dve", lambda E, hp=hp: E.max(sv[:, hp, 8:16], s_w[:, hp, :]),
                         reads=[("s_w", hp)], writes=[("sv", hp, 1)])
                    P.op("dve", lambda E, hp=hp: E.max_index(si[:, hp, 8:16], sv[:, hp, 8:16], s_w[:, hp, :]),
                         reads=[("s_w", hp), ("sv", hp, 1)], writes=[("si", hp, 1)])
                svk = [("sv", hp, z) for hp in range(16) for z in range(2)]
                sik = [("si", hp, z) for hp in range(16) for z in range(2)]
                P.op("dve", lambda E: E.tensor_copy(sif[:], si[:]), reads=sik, writes=["sif"])
                sv4 = sv[:].rearrange("p (h two) k -> p h two k", two=2)
                sif4 = sif[:].rearrange("p (h two) k -> p h two k", two=2)
                P.op("dve", lambda E: E.tensor_tensor(
                    cand[:].rearrange("p h (a b) -> p h a b", a=16),
                    sv4[:, :, 0, :].unsqueeze(3).to_broadcast([128, 8, 16, 16]),
                    sv4[:, :, 1, :].unsqueeze(2).to_broadcast([128, 8, 16, 16]), ALU.add),
                    reads=svk, writes=["cand"])
                for h in range(8):
                    P.op("dve", lambda E, h=h: E.max(cv[:, h, 0:8], cand[:, h, :]),
                         reads=["cand"], writes=[("cv", h, 0)])
                    P.op("dve", lambda E, h=h: E.max_index(ci[:, h, 0:8], cv[:, h, 0:8], cand[:, h, :]),
                         reads=["cand", ("cv", h, 0)], writes=[("ci", h, 0)])
                    P.op("dve", lambda E, h=h: E.match_replace(cand_w[:, h, :], cv[:, h, 0:8], cand[:, h, :], -1e30),
                         reads=["cand", ("cv", h, 0)], writes=[("cand_w", h)])
                    P.op("dve", lambda E, h=h: E.max(cv[:, h, 8:16], cand_w[:, h, :]),
                         reads=[("cand_w", h)], writes=[("cv", h, 1)])
                    P.op("dve", lambda E, h=h: E.max_index(ci[:, h, 8:16], cv[:, h, 8:16], cand_w[:, h, :]),
                         reads=[("cand_w", h), ("cv", h, 1)], writes=[("ci", h, 1)])
                cvk = [("cv", h, z) for h in range(8) for z in range(2)]
                cik = [("ci", h, z) for h in range(8) for z in range(2)]
                P.op("dve", lambda E: E.tensor_single_scalar(k12u[:, 0, :, :], ci[:], 4, ALU.logical_shift_right),
                     reads=cik, writes=["k1u"])
                P.op("dve", lambda E: E.tensor_single_scalar(k12u[:, 1, :, :], ci[:], 15, ALU.bitwise_and),
                     reads=cik, writes=["k2u"])
                P.op("dve", lambda E: E.tensor_copy(k12f[:], k12u[:]), reads=["k1u", "k2u"], writes=["k12f"])
                for z in range(2):
                    P.op("dve", lambda E, z=z: E.tensor_tensor(
                        eq[:], k12f[:, z, :, :].unsqueeze(3).to_broadcast([128, 8, 16, 16]),
                        iota16.unsqueeze(1).unsqueeze(1).to_broadcast([128, 8, 16, 16]), ALU.is_equal),
                        reads=["k12f", "iota_f"], writes=["eq"])
                    P.op("dve", lambda E, z=z: E.tensor_tensor(
                        eq[:], eq[:], sif4[:, :, z, :].unsqueeze(2).to_broadcast([128, 8, 16, 16]), ALU.mult),
                        reads=["eq", "sif"], writes=["eq"])
                    P.op("dve", lambda E, z=z: E.tensor_reduce(
                        IJ[:, z, :].rearrange("p (h k) -> p h k", h=8), eq[:], AX.X, ALU.add),
                        reads=["eq"], writes=[("IJ", z)])
                P.op("dve", lambda E: E.tensor_tensor(
                    cvm[:], cv[:], cv[:, :, 0:1].to_broadcast([128, 8, 16]), ALU.subtract),
                    reads=cvk, writes=["cvm"])
                P.op("act", lambda E: E.activation(cvm[:], cvm[:], AF.Exp), reads=["cvm"], writes=["cvm"])
                P.op("dve", lambda E: E.tensor_reduce(zs[:], cvm[:], AX.X, ALU.add), reads=["cvm"], writes=["zs"])
                P.op("dve", lambda E: E.reciprocal(rzs[:], zs[:]), reads=["zs"], writes=["rzs"])
                P.op("dve", lambda E: E.tensor_tensor(
                    IJ[:, 2, :].rearrange("p (h k) -> p h k", h=8), cvm[:],
                    rzs[:].unsqueeze(2).to_broadcast([128, 8, 16]), ALU.mult),
                    reads=["cvm", "rzs"], writes=[("IJ", 2)])
                for z in range(3):
                    P.op("pe", lambda E, z=z: E.transpose(ps_t[:, z, :], IJ[:, z, :], ident_f[:]),
                         reads=[("IJ", z), "ident_f"], writes=["ps_t"])
                P.op("act", lambda E: E.activation(IJT[:], ps_t[:], AF.Copy), reads=["ps_t"], writes=["IJT"])
                for t in range(128):
                    ab = t % NAB
                    P.op("dve", lambda E, t=t, ab=ab: E.tensor_scalar(
                        A_t[ab][:], iota_f[:], IJT[:, 0, t:t + 1], IJT[:, 2, t:t + 1], ALU.is_equal, ALU.mult),
                        reads=["IJT", "iota_f"], writes=[("A_t", ab)])
                    P.op("dve", lambda E, t=t, ab=ab: E.tensor_scalar(
                        B_t[ab][:], iota_f[:], IJT[:, 1, t:t + 1], None, ALU.is_equal),
                        reads=["IJT", "iota_f"], writes=[("B_t", ab)])
                    wb = (t // 4) % 2
                    P.op("pe", lambda E, t=t, ab=ab, wb=wb: E.matmul(
                        ps_w[wb][:, t % 4, :], B_t[ab][:], A_t[ab][:], start=True, stop=True),
                        reads=[("A_t", ab), ("B_t", ab)], writes=[("ps_w", wb)])
                    if t % 4 == 3:
                        t0 = t - 3
                        P.op("act", lambda E, t0=t0, wb=wb, wpar=wpar: E.activation(
                            WT[wpar][:, :, t0:t0 + 4].rearrange("j i t -> j t i"), ps_w[wb][:], AF.Copy),
                            reads=[("ps_w", wb)], writes=[("WT", wpar, t0 // 4)])
                if stop_after == "p3" and tb == NOWN - 1:
                    dIJ = dout("dbg_IJ", [128, 3, 128])
                    dsv = dout("dbg_sv", [128, 16, 16])
                    dxn = dout("dbg_xnT", [128, 16, TOK], BF16)
                    dcv = dout("dbg_cv", [128, 8, 16])
                    P.dma("sp", dIJ, IJ[:], reads=[("IJ", z) for z in range(3)], is_output=True)
                    P.dma("sp", dsv, sv[:], reads=svk, is_output=True)
                    P.dma("sp", dxn, xnT[:], reads=xn_keys, is_output=True)
                    P.dma("sp", dcv, cv[:], reads=cvk, is_output=True)
                for i8 in range(8):
                    P.dma("sp", wd_d[i8 * 16:(i8 + 1) * 16, :, tb * 128:(tb + 1) * 128].rearrange("i j t -> j i t"),
                          WT[wpar][:, i8 * 16:(i8 + 1) * 16, :],
                          reads=[("WT", wpar, q4) for q4 in range(32)], writes=[("wd_d", tb, i8)])
    wd_keys = [("wd_d", tb, i8) for tb in range(8) for i8 in range(8)]
    if stop_after == "p3":
        return finish_dbg(nc, P, out, [])

    with Phase() as ph:
        acc = sb(ph, "acc", [128, NOWN, D], F32)
        P.dma("sp", acc[:], hs1_d.rearrange("(b p) d -> p b d", p=128), reads=hs1_keys, writes=["acc"])
        NU = 2
        u_c = [sb(ph, f"u_c{i}", [128, D], BF16) for i in range(NU)]
        uT_c = [sb(ph, f"uT_c{i}", [128, 16, 128], BF16) for i in range(NU)]
        NV = GRP + 2
        v_c = [sb(ph, f"v_c{i}", [128, D], BF16) for i in range(NV)]
        wt_c = [sb(ph, f"wt_c{i}", [128, TOK], BF16) for i in range(2)]
        ga = [sb(ph, f"ga{i}", [128, TOK], BF16) for i in range(2)]
        NL = 2 * GRP
        lh = [sb(ph, f"lh{i}", [128, TOK], BF16) for i in range(NL)]
        ptp = [bf(0), bf(1)]
        pa = [BK[2], BK[3]]
        pout = [BK[4], BK[5], BK[6], BK[7]]

        def load_chunk(ci_):
            ui = ci_ % NU
            vi = ci_ % NV
            wi = ci_ % 2
            for hh in range(2):
                P.dma("pool", u_c[ui][:, hh * 1024:(hh + 1) * 1024], pu[ci_ * 128:(ci_ + 1) * 128, hh * 1024:(hh + 1) * 1024],
                      writes=[("u_c", ui, hh)])
                P.dma("pool", v_c[vi][:, hh * 1024:(hh + 1) * 1024], pv[ci_ * 128:(ci_ + 1) * 128, hh * 1024:(hh + 1) * 1024],
                      writes=[("v_c", vi, hh)])
            P.dma("sp", wt_c[wi][:], wd_d[ci_], reads=wd_keys, writes=[("wt_c", wi)])

        load_chunk(0)
        for ci_ in range(NCH):
            if ci_ + 1 < NCH:
                load_chunk(ci_ + 1)
            ui = ci_ % NU
            vi = ci_ % NV
            wi = ci_ % 2
            li = ci_ % NL
            gi = ci_ % 2
            for h8 in range(2):
                for j in range(8):
                    c = h8 * 8 + j
                    P.op("pe", lambda E, h8=h8, j=j, c=c, ui=ui: E.transpose(
                        ptp[h8][:, j, :], u_c[ui][:, c * 128:(c + 1) * 128], ident_bf[:]),
                        reads=[("u_c", ui, h8), "ident_bf"], writes=[("qtp", h8)])
                if h8 == 0:
                    P.op("act", lambda E, h8=h8, ui=ui: E.activation(
                        uT_c[ui][:, h8 * 8:(h8 + 1) * 8, :], ptp[h8][:], AF.Copy),
                        reads=[("qtp", h8)], writes=[("uT_c", ui, h8)])
                else:
                    P.op("dve", lambda E, h8=h8, ui=ui: E.tensor_copy(
                        uT_c[ui][:, h8 * 8:(h8 + 1) * 8, :], ptp[h8][:]),
                        reads=[("qtp", h8)], writes=[("uT_c", ui, h8)])
            for tg in range(2):
                for c in range(16):
                    P.op("pe", lambda E, tg=tg, c=c, ui=ui: E.matmul(
                        pa[tg][:], uT_c[ui][:, c, :], xnT[:, c, tg * 512:(tg + 1) * 512],
                        start=(c == 0), stop=(c == 15)),
                        reads=[("uT_c", ui, c // 8)] + xn_keys, writes=[("qa", tg)])
                P.op("act", lambda E, tg=tg, gi=gi: E.activation(
                    ga[gi][:, tg * 512:(tg + 1) * 512], pa[tg][:], AF.Gelu),
                    reads=[("qa", tg)], writes=[("ga", gi, tg)])
            P.op("dve", lambda E, gi=gi, wi=wi, li=li: E.tensor_tensor(lh[li][:], ga[gi][:], wt_c[wi][:], ALU.mult),
                 reads=[("ga", gi, 0), ("ga", gi, 1), ("wt_c", wi)], writes=[("lh", li)])
            if ci_ % GRP == GRP - 1:
                cs_ = list(range(ci_ - GRP + 1, ci_ + 1))
                for tb in range(NOWN):
                    for ng in range(4):
                        for k_, cc in enumerate(cs_):
                            P.op("pe", lambda E, tb=tb, ng=ng, cc=cc, k_=k_: E.matmul(
                                pout[ng][:], lh[cc % NL][:, tb * 128:(tb + 1) * 128],
                                v_c[cc % NV][:, ng * 512:(ng + 1) * 512],
                                start=(k_ == 0), stop=(k_ == GRP - 1)),
                                reads=[("lh", cc % NL), ("v_c", cc % NV, ng // 2)], writes=[("qo", ng)])
                        P.op("dve", lambda E, tb=tb, ng=ng: E.tensor_tensor(
                            acc[:, tb, ng * 512:(ng + 1) * 512], pout[ng][:], acc[:, tb, ng * 512:(ng + 1) * 512],
                            ALU.add),
                            reads=[("qo", ng), "acc", ("acc", tb, ng)], writes=[("acc", tb, ng)])
        gfin = sb(ph, "gfin", [128, D], F32)
        P.dma("sp", gfin[:], gvec[2:3, :].partition_broadcast(128), writes=["gfin"])
        outb = [sb(ph, f"outb{i}", [128, D], F32) for i in range(2)]
        tmpf = (sb(ph, "fss", [128, 2]), sb(ph, "frt", [128, 2]), sb(ph, "frstd", [128, 2]))
        junk2 = sb(ph, "junk2", [128, D], BF16)
        for ob in range(NOWN):
            i = ob % 2
            norm_rows(acc[:, ob, :], [("acc", ob, ng) for ng in range(4)], gfin, "gfin", outb[i][:], ("outb", i), junk2, tmpf, i)
            P.dma("sp", out[ob * 128:(ob + 1) * 128, :], outb[i][:], reads=[("outb", i)], writes=[("out", ob)],
                  is_output=True)
    peer.close()
    top.close()
    P.finish()
    P.emit()
    return nc


def finish_dbg(nc, P, out, stacks):
    scr = DBG_SCR[0]
    P.barrier()
    for k, (sid, t, isps) in enumerate(LIVE):
        ap = t[:]
        while len(ap.shape) > 2:
            ap = ap[:, 0]
        if isps:
            P.op("dve", lambda E, ap=ap: E.tensor_copy(scr[0:1, 32:33], ap[0:1, 0:1]), writes=[("lv", k)])
        else:
            P.op("dve", lambda E, ap=ap: E.tensor_copy(scr[0:1, 32:33], ap[0:1, 0:1]), writes=[("lv", k)])
    for i, ap in enumerate(ALL_INPUTS):
        flat = ap
        while len(flat.shape) > 2:
            flat = flat[0]
        n = min(16, flat.shape[1])
        P.dma("sp", scr[0:1, 0:n], flat[0:1, 0:n], writes=[("touch", i)])
    P.dma("sp", out[0:1, 0:64], scr[0:1, 0:64], reads=[("touch", i) for i in range(len(ALL_INPUTS))], is_output=True)
    P.finish()
    P.emit()
    return nc


def _consts():
    idx = np.arange(128)
    same = (idx[:, None] // 64) == (idx[None, :] // 64)
    s, t = idx[:, None], idx[None, :]
    triX = (same & (s <= t)).astype(np.float32)
    triY = (same & (s >= t)).astype(np.float32)
    aftX = (same & (s > t)).astype(np.float32)
    aftY = (same & (s < t)).astype(np.float32)
    tri = np.stack([triX, triY, aftX, aftY], 0)
    ind2 = np.zeros((128, 2), np.float32)
    ind2[:64, 0] = 1.0
    ind2[64:, 1] = 1.0
    return tri, ind2


def _core_inputs(inp, b, half, shared):
    x = inp["x"][b]
    meta = inp["meta_tokens"]
    xs = np.zeros((S, D), np.float32)
    pos = np.zeros(S, np.float64)
    valid = np.zeros(S, np.float32)
    ar = np.arange(1024)
    if half == 0:
        xs[112:128] = meta
        pos[112:128] = np.arange(16)
        valid[112:128] = 1
        xs[128:1152] = x[0:1024]
        pos[128:1152] = 16 + ar
        xs[1152:2176] = x[1024:2048]
        pos[1152:2176] = 16 + 1024 + ar
        valid[128:2176] = 1
    else:
        xs[128:1152] = x[2047:1023:-1]
        pos[128:1152] = 16 + 2047 - ar
        xs[1152:2176] = x[1023::-1]
        pos[1152:2176] = 16 + 1023 - ar
        xs[2176:2192] = meta[::-1]
        pos[2176:2192] = 15 - np.arange(16)
        valid[128:2192] = 1
    hf = 16
    inv = 1.0 / (500000.0 ** (np.arange(hf, dtype=np.float32) / hf))
    ang = pos.astype(np.float32)[:, None] * inv[None, :].astype(np.float32)
    cos = np.cos(ang).astype(np.float32)
    sin = np.sin(ang).astype(np.float32)
    cs = np.stack([cos.reshape(NB, 128, 16).transpose(1, 0, 2), sin.reshape(NB, 128, 16).transpose(1, 0, 2)], 0)
    w_in = inp["w_in"][0]
    lr_f = w_in[:, OFF_LR:OFF_LR + 16]
    lr_b = w_in[:, OFF_LR + 16:OFF_LR + 32]
    fw = np.concatenate([inp["gla_w2_fwd"][0], inp["gla_b_fwd"][0][None]], 0)
    bw = np.concatenate([inp["gla_w2_bwd"][0], inp["gla_b_bwd"][0][None]], 0)
    idx = np.arange(64)
    s, t = idx[:, None], idx[None, :]
    if half == 0:
        w_lr = np.concatenate([lr_f, lr_b], 1)
        w2xy = np.stack([fw, bw], 0)
        mX = (s <= t)
        mY = (s > t)
    else:
        w_lr = np.concatenate([lr_b, lr_f], 1)
        w2xy = np.stack([bw, fw], 0)
        mX = (s < t)
        mY = (s >= t)
    masks = np.stack([np.concatenate([mX, mX], 0), np.concatenate([mY, mY], 0)], 0).astype(np.float32)
    d = dict(shared)
    d.update({
        "xs": xs,
        "w_lr": np.ascontiguousarray(w_lr),
        "w2xy": np.ascontiguousarray(w2xy),
        "masks": masks,
        "cs": np.ascontiguousarray(cs),
        "valid": np.ascontiguousarray(valid.reshape(NB, 128).T),
    })
    return d


def _shared_inputs(inp):
    tri, ind2 = _consts()
    return {
        "w_in": np.ascontiguousarray(inp["w_in"][0]),
        "gvec": np.ascontiguousarray(np.stack([inp["g_mix"][0], inp["g_ffn"][0], inp["g_final"]], 0)),
        "gla_gn": np.ascontiguousarray(inp["gla_g_norm"][0][None]),
        "diff_gn": np.ascontiguousarray(inp["diff_g_norm"][0][None]),
        "lamv": np.ascontiguousarray(np.stack([inp["diff_lq1"][0], inp["diff_lk1"][0], inp["diff_lq2"][0],
                                               inp["diff_lk2"][0]], 0)),
        "tri": tri,
        "ind2": ind2,
        "w_a": np.ascontiguousarray(inp["w_branch_gla"][0]),
        "w_b": np.ascontiguousarray(inp["w_branch_diff"][0]),
        "w_o": np.ascontiguousarray(inp["w_out"][0]),
        "w_q": np.ascontiguousarray(inp["peer_w_q"][0]),
        "subk": np.ascontiguousarray(inp["peer_sub_keys"][0].reshape(16, 128, 128)),
        "pu": np.ascontiguousarray(inp["peer_u"][0]),
        "pv": np.ascontiguousarray(inp["peer_v"][0]),
    }


def make_in_maps(inputs):
    inp = {k: np.asarray(v, dtype=np.float32) for k, v in inputs.items()}
    shared = _shared_inputs(inp)
    return [_core_inputs(inp, c // 2, c % 2, shared) for c in range(8)]


def assemble(results, key="out"):
    o = np.zeros((4, 2048, D), np.float32)
    for c in range(8):
        b, half = c // 2, c % 2
        r = np.asarray(results[c][key])
        if half == 0:
            o[b, 0:1024] = r
        else:
            o[b, 1024:2048] = r[::-1]
    return o


def kernel(**inputs):
    nc = build()
    in_maps = make_in_maps(inputs)
    res = run_bass_kernel_spmd(nc, in_maps, core_ids=list(range(8)))
    return assemble(res.results)
```
